# Optimizing a Trainium2 kernel written in Bass

```python
import math
import jax, jax.numpy as jnp
from jax import lax
import numpy as np

D_MODEL = 1024
BATCH = 32
SEQ = 2048
DEPTH = 1

CHUNK = 64
Q_BLOCK = 128
EPS = 1e-6
D_MIX = D_MODEL
M_HEADS = 4
M_WIDTH = D_MIX // 2
M_HEAD_DIM = M_WIDTH // M_HEADS
CONV_WIDTH = 4
A_HEADS = 4
A_WIDTH = D_MIX - M_WIDTH
A_HEAD_DIM = A_WIDTH // A_HEADS
A_QK_DIM = A_HEAD_DIM // 2
IN_SIZES = (2 * M_WIDTH, M_WIDTH, M_WIDTH, M_HEADS, M_HEADS, A_WIDTH, A_WIDTH, A_WIDTH)
IN_COLS = sum(IN_SIZES)
IN_SPLITS = tuple(int(s) for s in np.cumsum(IN_SIZES)[:-1])
N_GROUPS = 4
EXPERTS_PER_GROUP = 8
N_EXPERTS = N_GROUPS * EXPERTS_PER_GROUP
TOP_K = 2
D_EXPERT = D_MODEL // 2
MOE_BLOCK = 256

kernel_name = 'hymba_mlstm_diffattn_hmoe_block'


def rms_norm(x, w):
    xf = x.astype(jnp.float32)
    y = xf * lax.rsqrt(jnp.mean(xf * xf, axis=-1, keepdims=True) + EPS)
    return (y * w.astype(jnp.float32)).astype(x.dtype)


def causal_conv(x, w, b):
    S = x.shape[1]
    xp = jnp.pad(x, ((0, 0), (CONV_WIDTH - 1, 0), (0, 0)))
    out = b
    for j in range(CONV_WIDTH):
        out = out + xp[:, j:j + S] * w[j]
    return out


def mlstm_chunkwise(q, k, v, i_pre, f_pre):
    Bn, S, H, Dh = q.shape
    NC = S // CHUNK

    def to_chunks(t):
        t = t.astype(jnp.float32).reshape((Bn, NC, CHUNK) + t.shape[2:])
        return jnp.moveaxis(jnp.moveaxis(t, 1, 0), 3, 2)

    qc = to_chunks(q)
    kc = to_chunks(k) * (Dh ** -0.5)
    vc = to_chunks(v)
    ic = to_chunks(i_pre)
    fc = jax.nn.log_sigmoid(to_chunks(f_pre))
    causal = jnp.tril(jnp.ones((CHUNK, CHUNK), dtype=bool))

    def step(carry, inp):
        C, n, m = carry
        q_c, k_c, v_c, i_c, lf_c = inp
        b = jnp.cumsum(lf_c, axis=-1)
        log_d = jnp.where(causal, b[..., :, None] - b[..., None, :] + i_c[..., None, :], -jnp.inf)
        log_inter = b + m[..., None]
        m_t = jnp.maximum(log_inter, jnp.max(log_d, axis=-1))
        d = jnp.exp(log_d - m_t[..., None])
        inter = jnp.exp(log_inter - m_t)
        s = jnp.einsum('bhtd,bhsd->bhts', q_c, k_c) * d
        num = jnp.einsum('bhts,bhse->bhte', s, v_c) + inter[..., None] * jnp.einsum('bhtd,bhde->bhte', q_c, C)
        den = jnp.sum(s, axis=-1) + inter * jnp.einsum('bhtd,bhd->bht', q_c, n)
        h = num / jnp.maximum(jnp.abs(den), jnp.exp(-m_t))[..., None]
        b_end = b[..., -1]
        log_w = b_end[..., None] - b + i_c
        m_new = jnp.maximum(b_end + m, jnp.max(log_w, axis=-1))
        w = jnp.exp(log_w - m_new[..., None])
        decay = jnp.exp(b_end + m - m_new)
        C_new = decay[..., None, None] * C + jnp.einsum('bhs,bhsd,bhse->bhde', w, k_c, v_c)
        n_new = decay[..., None] * n + jnp.einsum('bhs,bhsd->bhd', w, k_c)
        return (C_new, n_new, m_new), h

    init = (jnp.zeros((Bn, H, Dh, Dh), jnp.float32),
            jnp.zeros((Bn, H, Dh), jnp.float32),
            jnp.zeros((Bn, H), jnp.float32))
    _, h = lax.scan(step, init, (qc, kc, vc, ic, fc))
    h = jnp.moveaxis(jnp.moveaxis(h, 0, 1), 2, 3)
    return h.reshape(Bn, S, H, Dh)


def diff_attention(q, k, v, q_norm_w, k_norm_w, lq1, lk1, lq2, lk2, a_norm_w, lambda_init):
    Bn, S = q.shape[:2]
    NQ = S // Q_BLOCK
    q = rms_norm(q, q_norm_w)
    k = rms_norm(k, k_norm_w)
    f32 = jnp.float32
    lam = (jnp.exp(jnp.sum(lq1.astype(f32) * lk1.astype(f32)))
           - jnp.exp(jnp.sum(lq2.astype(f32) * lk2.astype(f32))) + lambda_init)
    kt = jnp.transpose(k, (0, 2, 3, 1, 4))
    vt = jnp.transpose(v, (0, 2, 1, 3))
    qb = q.reshape(Bn, NQ, Q_BLOCK, A_HEADS, 2, A_QK_DIM).transpose(1, 0, 3, 4, 2, 5)
    key_chunk = jnp.arange(S) // CHUNK
    scale = A_QK_DIM ** -0.5

    def block(args):
        q_blk, idx = args
        q_chunk = (idx * Q_BLOCK + jnp.arange(Q_BLOCK)) // CHUNK
        mask = key_chunk[None, :] <= q_chunk[:, None]
        s = jnp.einsum('bhcqd,bhcsd->bhcqs', q_blk, kt).astype(f32) * scale
        p = jax.nn.softmax(jnp.where(mask, s, -jnp.inf), axis=-1)
        attn = p[:, :, 0] - lam * p[:, :, 1]
        return jnp.einsum('bhqs,bhsd->bhqd', attn.astype(v.dtype), vt)

    o = lax.map(block, (qb, jnp.arange(NQ)))
    o = o.transpose(1, 0, 3, 2, 4).reshape(Bn, S, A_HEADS, A_HEAD_DIM)
    o = rms_norm(o, a_norm_w) * (1.0 - lambda_init)
    return o.reshape(Bn, S, A_WIDTH)


def hybrid_mixer(h, w_in, b_igate, b_fgate, conv_w, conv_b, m_norm_w, q_norm_w, k_norm_w,
                 lq1, lk1, lq2, lk2, a_norm_w, w_out, lambda_init):
    Bn, S, _ = h.shape
    proj = h @ w_in
    mqk, mv, mo, mi, mf, aq, ak, av = jnp.split(proj, IN_SPLITS, axis=-1)
    qk = jax.nn.silu(causal_conv(mqk, conv_w, conv_b))
    mq, mk = jnp.split(qk, 2, axis=-1)
    shp = (Bn, S, M_HEADS, M_HEAD_DIM)
    hm = mlstm_chunkwise(mq.reshape(shp), mk.reshape(shp), mv.reshape(shp), mi + b_igate, mf + b_fgate)
    hm = jax.nn.sigmoid(mo).reshape(shp) * hm.astype(h.dtype)
    hm = rms_norm(hm, m_norm_w).reshape(Bn, S, M_WIDTH)
    ha = diff_attention(aq.reshape(Bn, S, A_HEADS, 2, A_QK_DIM), ak.reshape(Bn, S, A_HEADS, 2, A_QK_DIM),
                        av.reshape(Bn, S, A_HEADS, A_HEAD_DIM), q_norm_w, k_norm_w,
                        lq1, lk1, lq2, lk2, a_norm_w, lambda_init)
    return jnp.concatenate([hm, ha], axis=-1) @ w_out


def hierarchical_moe(h, w_group, b_group, w_expert, b_expert, w_gate, w_up, w_down):
    Bn, S, D = h.shape
    T = Bn * S
    xt = h.reshape(T, D)
    f32 = jnp.float32
    g_logits = (xt @ w_group).astype(f32) + b_group.astype(f32)
    g_prob = jax.nn.softmax(g_logits, axis=-1)
    g_sel = jnp.argmax(g_logits, axis=-1)
    g_w = jnp.take_along_axis(g_prob, g_sel[:, None], axis=1)[:, 0]
    e_logits = ((xt @ w_expert).astype(f32) + b_expert.astype(f32)).reshape(T, N_GROUPS, EXPERTS_PER_GROUP)
    e_in = jnp.take_along_axis(e_logits, g_sel[:, None, None], axis=1)[:, 0]
    top_vals, top_idx = lax.top_k(e_in, TOP_K)
    weights = g_w[:, None] * jax.nn.softmax(top_vals, axis=-1)
    eid = (g_sel[:, None] * EXPERTS_PER_GROUP + top_idx).reshape(-1)
    A = T * TOP_K
    tok = jnp.arange(A) // TOP_K
    counts = jax.ops.segment_sum(jnp.ones_like(eid), eid, num_segments=N_EXPERTS)
    padded = ((counts + MOE_BLOCK - 1) // MOE_BLOCK) * MOE_BLOCK
    pad_end = jnp.cumsum(padded)
    pad_start = pad_end - padded
    start = jnp.cumsum(counts) - counts
    order = jnp.argsort(eid)
    eid_sorted = eid[order]
    dest_sorted = pad_start[eid_sorted] + (jnp.arange(A) - start[eid_sorted])
    dest = jnp.zeros((A,), jnp.int32).at[order].set(dest_sorted.astype(jnp.int32))
    P = ((A + MOE_BLOCK - 1) // MOE_BLOCK) * MOE_BLOCK + N_EXPERTS * MOE_BLOCK
    NB = P // MOE_BLOCK
    block_start = jnp.arange(NB) * MOE_BLOCK
    block_expert = jnp.minimum(jnp.sum(pad_end[None, :] <= block_start[:, None], axis=1), N_EXPERTS - 1)
    x_buf = jnp.zeros((P, D), h.dtype).at[dest].set(xt[tok])

    def expert_block(args):
        xb, e = args
        hid = jax.nn.silu(xb @ w_gate[e]) * (xb @ w_up[e])
        return hid @ w_down[e]

    y_buf = lax.map(expert_block, (x_buf.reshape(NB, MOE_BLOCK, D), block_expert)).reshape(P, D)
    y = jnp.sum(y_buf[dest].reshape(T, TOP_K, D) * weights[..., None].astype(h.dtype), axis=1)
    return y.reshape(Bn, S, D)


def setup_inputs(seed: int = 0) -> dict:
    key = jax.random.key(seed)
    ks = jax.random.split(key, 24)
    nrm = jax.random.normal
    f32 = jnp.float32
    L = DEPTH
    gain = lambda k, n: 1.0 + 0.01 * nrm(k, (L, n), f32)
    return {
        'x': nrm(ks[0], (BATCH, SEQ, D_MODEL), f32),
        'norm1_w': gain(ks[1], D_MODEL),
        'w_in': nrm(ks[2], (L, D_MODEL, IN_COLS), f32) * D_MODEL ** -0.5,
        'b_igate': 0.1 * nrm(ks[3], (L, M_HEADS), f32),
        'b_fgate': jnp.linspace(3.0, 6.0, M_HEADS, dtype=f32)[None, :] + 0.1 * nrm(ks[4], (L, M_HEADS), f32),
        'conv_w': nrm(ks[5], (L, CONV_WIDTH, 2 * M_WIDTH), f32) * CONV_WIDTH ** -0.5,
        'conv_b': 0.01 * nrm(ks[6], (L, 2 * M_WIDTH), f32),
        'm_norm_w': gain(ks[7], M_HEAD_DIM),
        'q_norm_w': gain(ks[8], A_QK_DIM),
        'k_norm_w': gain(ks[9], A_QK_DIM),
        'lambda_q1': 0.1 * nrm(ks[10], (L, A_QK_DIM), f32),
        'lambda_k1': 0.1 * nrm(ks[11], (L, A_QK_DIM), f32),
        'lambda_q2': 0.1 * nrm(ks[12], (L, A_QK_DIM), f32),
        'lambda_k2': 0.1 * nrm(ks[13], (L, A_QK_DIM), f32),
        'a_norm_w': gain(ks[14], A_HEAD_DIM),
        'w_out': nrm(ks[15], (L, D_MIX, D_MODEL), f32) * D_MIX ** -0.5,
        'norm2_w': gain(ks[16], D_MODEL),
        'w_group': nrm(ks[17], (L, D_MODEL, N_GROUPS), f32) * D_MODEL ** -0.5,
        'b_group': 0.01 * nrm(ks[18], (L, N_GROUPS), f32),
        'w_expert': nrm(ks[19], (L, D_MODEL, N_EXPERTS), f32) * D_MODEL ** -0.5,
        'b_expert': 0.01 * nrm(ks[20], (L, N_EXPERTS), f32),
        'w_gate': nrm(ks[21], (L, N_EXPERTS, D_MODEL, D_EXPERT), f32) * D_MODEL ** -0.5,
        'w_up': nrm(ks[22], (L, N_EXPERTS, D_MODEL, D_EXPERT), f32) * D_MODEL ** -0.5,
        'w_down': nrm(ks[23], (L, N_EXPERTS, D_EXPERT, D_MODEL), f32) * D_EXPERT ** -0.5,
    }


def reference(x, norm1_w, w_in, b_igate, b_fgate, conv_w, conv_b, m_norm_w, q_norm_w, k_norm_w,
              lambda_q1, lambda_k1, lambda_q2, lambda_k2, a_norm_w, w_out, norm2_w,
              w_group, b_group, w_expert, b_expert, w_gate, w_up, w_down):
    for l in range(DEPTH):
        lambda_init = 0.8 - 0.6 * math.exp(-0.3 * l)
        h = rms_norm(x, norm1_w[l])
        x = x + hybrid_mixer(h, w_in[l], b_igate[l], b_fgate[l], conv_w[l], conv_b[l], m_norm_w[l],
                             q_norm_w[l], k_norm_w[l], lambda_q1[l], lambda_k1[l], lambda_q2[l],
                             lambda_k2[l], a_norm_w[l], w_out[l], lambda_init)
        h = rms_norm(x, norm2_w[l])
        x = x + hierarchical_moe(h, w_group[l], b_group[l], w_expert[l], b_expert[l],
                                 w_gate[l], w_up[l], w_down[l])
    return x
```

```python
import math
import os
from contextlib import ExitStack
import numpy as np
import concourse.bass as bass
import concourse.mybir as mybir
from concourse.bass_utils import run_bass_kernel_spmd

F32 = mybir.dt.float32
BF16 = mybir.dt.bfloat16
I32 = mybir.dt.int32
ALU = mybir.AluOpType
AF = mybir.ActivationFunctionType
AX = mybir.AxisListType
EPS = 1e-6
BLK = 256


class Trk:
    EPOCH = int(os.environ.get("TRK_EPOCH", "30000"))

    def __init__(self, nc):
        self.nc = nc
        self.E = dict(pe=nc.tensor, act=nc.scalar, dve=nc.vector, pool=nc.gpsimd, sp=nc.sync)
        self.S = []
        self.sem = {}
        self.cnt = {}
        self.final = {}
        for e in self.E:
            self._newsem(e)
        self.waited = {}
        self.lastw = {}
        self.readers = {}
        self.dsems = {q: [self._alloc("d%s%d" % (q, i)) for i in range(8)] for q in ("sp", "act", "pool")}
        self.dcnt = {}
        self.drr = {q: 0 for q in self.dsems}
        self.ninst = 0

    def _alloc(self, name):
        s = self.nc.semaphore(name).__enter__()
        self.S.append(s)
        return len(self.S) - 1

    def _newsem(self, e):
        if e in self.sem:
            self.final[self.sem[e]] = (self.cnt[e], e)
        self.sem[e] = self._alloc("e%s%d" % (e, len(self.S)))
        self.cnt[e] = 0

    def _wait(self, eng, tk, force=False):
        si, val, peng = tk
        if peng == eng == "pe" and not force:
            return
        if self.waited.get((eng, si), 0) >= val:
            return
        self.waited[(eng, si)] = val
        self.E[eng].wait_ge(self.S[si], val)
        self.ninst += 1

    def _deps(self, eng, r, w):
        for k in r:
            t = self.lastw.get(k)
            if t:
                self._wait(eng, t)
        for k in w:
            t = self.lastw.get(k)
            if t:
                self._wait(eng, t)
            for t in self.readers.get(k, {}).values():
                self._wait(eng, t)

    def _post(self, tk, r, w):
        for k in w:
            self.lastw[k] = tk
            self.readers[k] = {}
        for k in r:
            self.readers.setdefault(k, {})[tk[0]] = tk

    def op(self, eng, fn, r=(), w=()):
        self._deps(eng, r, w)
        inst = fn(self.E[eng])
        si = self.sem[eng]
        self.cnt[eng] += 1
        val = self.cnt[eng]
        inst.then_inc(self.S[si], 1)
        self.ninst += 1
        tk = (si, val, eng)
        self._post(tk, r, w)
        if val >= self.EPOCH:
            self._newsem(eng)
        return tk

    def pe(self, fn, r=(), w=()):
        return self.op("pe", fn, r, w)

    def act(self, fn, r=(), w=()):
        return self.op("act", fn, r, w)

    def dve(self, fn, r=(), w=()):
        return self.op("dve", fn, r, w)

    def pool(self, fn, r=(), w=()):
        return self.op("pool", fn, r, w)

    def dma(self, q, fn, r=(), w=()):
        self._deps(q, r, w)
        lst = self.dsems[q]
        i = self.drr[q]
        self.drr[q] = (i + 1) % len(lst)
        si = lst[i]
        prev = self.dcnt.get(si, 0)
        if prev:
            self._wait(q, (si, prev, "dma"))
        inst = fn(self.E[q])
        self.dcnt[si] = prev + 16
        inst.then_inc(self.S[si], 16)
        self.ninst += 1
        tk = (si, prev + 16, "dma")
        self._post(tk, r, w)
        return tk

    def barrier(self, engines=None):
        latest = []
        for si, (c, e) in self.final.items():
            if c > 0:
                latest.append((si, c, e))
        for e in self.E:
            if self.cnt[e] > 0:
                latest.append((self.sem[e], self.cnt[e], e))
        for si, c in self.dcnt.items():
            if c > 0:
                latest.append((si, c, "dma"))
        for eng in (engines or self.E):
            for t in latest:
                self._wait(eng, t, force=True)
        self.lastw.clear()
        self.readers.clear()


def build(NSEQ=4, S=2048, dbg=False):
    T = NSEQ * S
    NT = T // 128
    TPS = S // 128
    A = 2 * T
    B = BLK
    MAXB = A // B
    NB = A // B + 32
    NSLOT = NB * B
    NG = NSLOT // 128
    GB = B // 128
    NE = 32
    LAM_INIT = 0.8 - 0.6 * math.exp(-0.3 * 0)

    nc = bass.Bass("TRN2", target_bir_lowering=False)

    def din(name, shape, dt=F32):
        return nc.dram_tensor(name, shape, dt, kind="ExternalInput").ap()

    x = din("x", [T, 1024])
    norm1_w = din("norm1_w", [1, 1024])
    w_in = din("w_in", [1, 1024, 3592])
    b_igate = din("b_igate", [1, 4])
    b_fgate = din("b_fgate", [1, 4])
    conv_w = din("conv_w", [1, 4, 1024])
    conv_b = din("conv_b", [1, 1024])
    m_norm_w = din("m_norm_w", [1, 128])
    q_norm_w = din("q_norm_w", [1, 64])
    k_norm_w = din("k_norm_w", [1, 64])
    lq1 = din("lambda_q1", [1, 64])
    lk1 = din("lambda_k1", [1, 64])
    lq2 = din("lambda_q2", [1, 64])
    lk2 = din("lambda_k2", [1, 64])
    a_norm_w = din("a_norm_w", [1, 128])
    w_out = din("w_out", [1, 1024, 1024])
    norm2_w = din("norm2_w", [1, 1024])
    w_group = din("w_group", [1, 1024, 4])
    b_group = din("b_group", [1, 4])
    w_expert = din("w_expert", [1, 1024, 32])
    b_expert = din("b_expert", [1, 32])
    w_gate = din("w_gate", [1, 32, 1024, 512])
    w_up = din("w_up", [1, 32, 1024, 512])
    w_down = din("w_down", [1, 32, 512, 1024])
    out = nc.dram_tensor("out", [T, 1024], F32, kind="ExternalOutput").ap()
    kd = "ExternalOutput" if dbg else "Internal"
    x1buf = nc.dram_tensor("x1buf", [T, 1024], F32, kind=kd).ap()
    h2buf = nc.dram_tensor("h2buf", [T + 128, 1024], BF16, kind=kd).ap()
    wbf = nc.dram_tensor("wbf", [NE * 128, 12288], BF16, kind="Internal").ap()
    slot_tok = nc.dram_tensor("slot_tok", [NSLOT, 16], I32, kind=kd).ap()
    ybuf = nc.dram_tensor("ybuf", [NSLOT, 1024], F32, kind=kd).ap()
    if dbg:
        dbg_r = nc.dram_tensor("dbg_r", [128, NT * 4], F32, kind="ExternalOutput").ap()
        dbg_d = nc.dram_tensor("dbg_d", [128, NT * 2], F32, kind="ExternalOutput").ap()
        dbg_be = nc.dram_tensor("dbg_be", [128, NB], F32, kind="ExternalOutput").ap()

    tk = Trk(nc)
    bnds = {}
    for nm, val in (("slot", NSLOT - 1), ("w", NE * 128 - 1), ("h", T + 127)):
        rg = nc.gpsimd.register("bnd_" + nm).__enter__()
        nc.gpsimd.reg_mov(rg, val)
        bnds[nm] = nc.gpsimd.snap(rg)
    ps = [nc.psum_tensor("ps%d" % i, [128, 512], F32).__enter__() for i in range(8)]
    rot = [0]

    def bank():
        i = rot[0]
        rot[0] = (i + 1) % 6
        return i

    def sbt(stack, name, shape, dt):
        return stack.enter_context(nc.sbuf_tensor(name, shape, dt))

    cs = ExitStack()
    ident_f = sbt(cs, "ident_f", [128, 128], F32)
    ident_b = sbt(cs, "ident_b", [128, 128], BF16)
    tri = sbt(cs, "tri", [128, 128], F32)
    ustrict = sbt(cs, "ustrict", [128, 128], BF16)
    ones_b = sbt(cs, "ones_b", [128, 128], BF16)
    zeros_f = sbt(cs, "zeros_f", [128, 128], F32)
    iota32 = sbt(cs, "iota32", [128, 32], F32)
    pidx = sbt(cs, "pidx", [128, 1], F32)
    n2w = sbt(cs, "n2w", [128, 1024], F32)
    wr_hi = sbt(cs, "wr_hi", [128, 8, 36], BF16)
    wr_lo = sbt(cs, "wr_lo", [128, 8, 36], BF16)
    rbias = sbt(cs, "rbias", [128, 36], F32)
    eid1 = sbt(cs, "eid1", [128, NT], F32)
    eid2 = sbt(cs, "eid2", [128, NT], F32)
    rw1 = sbt(cs, "rw1", [128, NT], F32)
    rw2 = sbt(cs, "rw2", [128, NT], F32)

    with ExitStack() as s0:
        colf = sbt(s0, "colf", [128, 128], F32)
        rowf = sbt(s0, "rowf", [128, 128], F32)
        wr_f = sbt(s0, "wr_f", [128, 8, 36], F32)
        wr_t = sbt(s0, "wr_t", [128, 8, 36], F32)
        tk.pool(lambda e: e.iota(colf[:], pattern=[[1, 128]], base=0, channel_multiplier=0,
                                 allow_small_or_imprecise_dtypes=True), w=["colf"])
        tk.pool(lambda e: e.iota(rowf[:], pattern=[[0, 128]], base=0, channel_multiplier=1,
                                 allow_small_or_imprecise_dtypes=True), w=["rowf"])
        tk.pool(lambda e: e.iota(iota32[:], pattern=[[1, 32]], base=0, channel_multiplier=0,
                                 allow_small_or_imprecise_dtypes=True), w=["c"])
        tk.pool(lambda e: e.iota(pidx[:], pattern=[[0, 1]], base=0, channel_multiplier=1,
                                 allow_small_or_imprecise_dtypes=True), w=["c2"])
        tk.dve(lambda e: e.tensor_tensor(out=ident_f[:], in0=colf[:], in1=rowf[:], op=ALU.is_equal),
               r=["colf", "rowf"], w=["ident_f"])
        tk.dve(lambda e: e.tensor_copy(out=ident_b[:], in_=ident_f[:]), r=["ident_f"], w=["ident_b"])
        tk.dve(lambda e: e.tensor_tensor(out=tri[:], in0=colf[:], in1=rowf[:], op=ALU.is_ge),
               r=["colf", "rowf"], w=["tri"])
        tk.dve(lambda e: e.tensor_tensor(out=ustrict[:], in0=colf[:], in1=rowf[:], op=ALU.is_gt),
               r=["colf", "rowf"], w=["ustrict"])
        tk.dve(lambda e: e.memset(ones_b[:], 1.0), w=["ones_b"])
        tk.dve(lambda e: e.memset(zeros_f[:], 0.0), w=["zeros_f"])
        tk.dma("sp", lambda e: e.dma_start(out=n2w[:], in_=norm2_w.partition_broadcast(128)), w=["n2w"])
        tk.dma("sp", lambda e: e.dma_start(out=rbias[:, 0:4], in_=b_group.partition_broadcast(128)), w=["rb0"])
        tk.dma("sp", lambda e: e.dma_start(out=rbias[:, 4:36], in_=b_expert.partition_broadcast(128)), w=["rb1"])
        tk.dma("sp", lambda e: e.dma_start(out=wr_f[:, :, 0:4],
                                           in_=w_group[0].rearrange("(kc p) g -> p kc g", p=128)), w=["wrf0"])
        tk.dma("sp", lambda e: e.dma_start(out=wr_f[:, :, 4:36],
                                           in_=w_expert[0].rearrange("(kc p) g -> p kc g", p=128)), w=["wrf1"])
        tk.dve(lambda e: e.tensor_copy(out=wr_hi[:], in_=wr_f[:]), r=["wrf0", "wrf1"], w=["wr_hi"])
        tk.dve(lambda e: e.tensor_copy(out=wr_t[:], in_=wr_hi[:]), r=["wr_hi"], w=["wr_t"])
        tk.dve(lambda e: e.tensor_tensor(out=wr_lo[:], in0=wr_f[:], in1=wr_t[:], op=ALU.subtract),
               r=["wr_t", "wrf0", "wrf1"], w=["wr_lo"])
        tk.barrier()

    with ExitStack() as sz:
        zrow = sbt(sz, "zrow", [128, 1024], BF16)
        tk.dve(lambda e: e.memset(zrow[:], 0.0), w=["zrow"])
        tk.dma("sp", lambda e: e.dma_start(out=h2buf[T:T + 128, :], in_=zrow[:]), r=["zrow"], w=["h2z"])
        tk.barrier()

    with ExitStack() as sa:
        win = sbt(sa, "win", [128, 8, 3592], BF16)
        wout = sbt(sa, "wout", [128, 8, 1024], BF16)
        convdiag = sbt(sa, "convdiag", [128, 32, 128], BF16)
        convb = sbt(sa, "convb", [128, 8], F32)
        nqk = sbt(sa, "nqk", [128, 2], F32)
        gbias = sbt(sa, "gbias", [4, 4], F32)
        neglam = sbt(sa, "neglam", [128, 1], F32)
        with ExitStack() as s1:
            stg = [sbt(s1, "stg%d" % i, [128, 3592], F32) for i in range(2)]
            n1 = sbt(s1, "n1", [128, 8], F32)
            cw = sbt(s1, "cw", [128, 4, 8], F32)
            mnw = sbt(s1, "mnw", [128, 1], F32)
            anw = sbt(s1, "anw", [128, 1], F32)
            lam4 = sbt(s1, "lam4", [128, 4, 64], F32)
            lamp = sbt(s1, "lamp", [128, 2, 64], F32)
            lams = sbt(s1, "lams", [128, 4], F32)
            tk.dma("sp", lambda e: e.dma_start(out=n1[:], in_=norm1_w.rearrange("o (kc p) -> p (o kc)", p=128),
                                               allow_slow_non_contiguous=True), w=["n1"])
            tk.dma("sp", lambda e: e.dma_start(out=cw[:], in_=conv_w[0].rearrange("j (c p) -> p j c", p=128),
                                               allow_slow_non_contiguous=True), w=["cw"])
            tk.dma("sp", lambda e: e.dma_start(out=convb[:], in_=conv_b.rearrange("o (c p) -> p (o c)", p=128),
                                               allow_slow_non_contiguous=True), w=["convb"])
            tk.dma("sp", lambda e: e.dma_start(out=mnw[:], in_=m_norm_w.rearrange("o p -> p o")), w=["mnw"])
            tk.dma("sp", lambda e: e.dma_start(out=anw[:], in_=a_norm_w.rearrange("o p -> p o")), w=["anw"])
            tk.dma("sp", lambda e: e.dma_start(out=nqk[0:64, 0:1], in_=q_norm_w.rearrange("o p -> p o")), w=["nqk0"])
            tk.dma("sp", lambda e: e.dma_start(out=nqk[64:128, 0:1], in_=q_norm_w.rearrange("o p -> p o")), w=["nqk1"])
            tk.dma("sp", lambda e: e.dma_start(out=nqk[0:64, 1:2], in_=k_norm_w.rearrange("o p -> p o")), w=["nqk2"])
            tk.dma("sp", lambda e: e.dma_start(out=nqk[64:128, 1:2], in_=k_norm_w.rearrange("o p -> p o")), w=["nqk3"])
            tk.dma("sp", lambda e: e.dma_start(out=gbias[:, 0:1], in_=b_igate.rearrange("o p -> p o")), w=["gb0"])
            tk.dma("sp", lambda e: e.dma_start(out=gbias[:, 1:2], in_=b_fgate.rearrange("o p -> p o")), w=["gb1"])
            for i, l in enumerate((lq1, lk1, lq2, lk2)):
                tk.dma("sp", lambda e, i=i, l=l: e.dma_start(out=lam4[:, i, :], in_=l.partition_broadcast(128)),
                       w=["lam4_%d" % i])
            tk.dve(lambda e: e.tensor_scalar(out=gbias[:, 2:3], in0=gbias[:, 1:2], scalar1=-1.0, scalar2=None,
                                             op0=ALU.mult), r=["gb1"], w=["gb2"])
            tk.dve(lambda e: e.tensor_scalar(out=anw[:], in0=anw[:], scalar1=1.0 - LAM_INIT, scalar2=None,
                                             op0=ALU.mult), r=["anw"], w=["anw"])
            tk.dve(lambda e: e.tensor_tensor(out=lamp[:, 0, :], in0=lam4[:, 0, :], in1=lam4[:, 1, :], op=ALU.mult),
                   r=["lam4_0", "lam4_1"], w=["lamp0"])
            tk.dve(lambda e: e.tensor_tensor(out=lamp[:, 1, :], in0=lam4[:, 2, :], in1=lam4[:, 3, :], op=ALU.mult),
                   r=["lam4_2", "lam4_3"], w=["lamp1"])
            tk.dve(lambda e: e.tensor_reduce(out=lams[:, 0:2], in_=lamp[:], axis=AX.X, op=ALU.add),
                   r=["lamp0", "lamp1"], w=["lams"])
            tk.act(lambda e: e.activation(out=lams[:, 2:4], in_=lams[:, 0:2], func=AF.Exp), r=["lams"], w=["lams2"])
            tk.dve(lambda e: e.scalar_tensor_tensor(out=neglam[:], in0=lams[:, 3:4], scalar=-LAM_INIT,
                                                    in1=lams[:, 2:3], op0=ALU.add, op1=ALU.subtract),
                   r=["lams2"], w=["neglam"])
            for j in range(4):
                for c in range(8):
                    tk.dve(lambda e, j=j, c=c: e.tensor_scalar(out=convdiag[:, j * 8 + c, :], in0=ident_f[:],
                                                               scalar1=cw[:, j, c:c + 1], scalar2=None, op0=ALU.mult),
                           r=["cw"], w=["cd%d_%d" % (j, c)])
            engs = ["dve", "act", "pool"]
            for kc in range(8):
                st = stg[kc % 2]
                sk = "stg%d" % (kc % 2)
                tk.dma("sp", lambda e, kc=kc, st=st: e.dma_start(out=st[:], in_=w_in[0, kc * 128:(kc + 1) * 128, :]),
                       w=[sk])
                for part in range(3):
                    lo, hi = part * 1200, min(3592, (part + 1) * 1200)
                    en = engs[part]
                    if en == "act":
                        tk.act(lambda e, kc=kc, st=st, lo=lo, hi=hi: e.activation(
                            out=win[:, kc, lo:hi], in_=st[:, lo:hi], func=AF.Copy, scale=n1[:, kc:kc + 1]),
                            r=[sk, "n1"], w=["win%d_%d" % (kc, part)])
                    else:
                        tk.op(en, lambda e, kc=kc, st=st, lo=lo, hi=hi: e.tensor_scalar(
                            out=win[:, kc, lo:hi], in0=st[:, lo:hi], scalar1=n1[:, kc:kc + 1], scalar2=None,
                            op0=ALU.mult), r=[sk, "n1"], w=["win%d_%d" % (kc, part)])
            for kc in range(8):
                st = stg[kc % 2]
                sk = "stg%d" % (kc % 2)
                tk.dma("sp", lambda e, kc=kc, st=st: e.dma_start(out=st[:, 0:1024],
                                                                 in_=w_out[0, kc * 128:(kc + 1) * 128, :]), w=[sk])
                sc = mnw if kc < 4 else anw
                tk.dve(lambda e, kc=kc, st=st, sc=sc: e.tensor_scalar(out=wout[:, kc, :], in0=st[:, 0:1024],
                                                                      scalar1=sc[:, 0:1], scalar2=None, op0=ALU.mult),
                       r=[sk, "mnw", "anw"], w=["wout%d" % kc])
            tk.barrier()

        xb = [sbt(sa, "xb%d" % i, [128, 1024], F32) for i in range(3)]
        junk = sbt(sa, "junk", [128, 1024], BF16)
        hb = sbt(sa, "hb", [128, 1024], BF16)
        hT = sbt(sa, "hT", [128, 8, 128], BF16)
        qkpre = sbt(sa, "qkpre", [128, 8, 131], BF16)
        qkT = sbt(sa, "qkT", [128, 8, 128], BF16)
        vm = sbt(sa, "vm", [128, 4, 129], BF16)
        og = sbt(sa, "og", [128, 512], F32)
        gt = sbt(sa, "gt", [4, 4, 128], F32)
        R = sbt(sa, "R", [4, 3, 128], F32)
        Rt = sbt(sa, "Rt", [4, 3, 128], F32)
        Rhi = sbt(sa, "Rhi", [4, 3, 128], BF16)
        Rlo = sbt(sa, "Rlo", [4, 3, 128], BF16)
        gcar = sbt(sa, "gcar", [4, 8], F32)
        gc = sbt(sa, "gc", [128, 12], F32)
        sTp = sbt(sa, "sTp", [128, 4, 128], BF16)
        kw = sbt(sa, "kw", [128, 4, 128], BF16)
        Cst = sbt(sa, "Cst", [128, 4, 129], F32)
        Cbf = sbt(sa, "Cbf", [128, 4, 129], BF16)
        mtmp = sbt(sa, "mtmp", [128, 16], F32)
        rtmp = sbt(sa, "rtmp", [128, 16], F32)
        dtmp = sbt(sa, "dtmp", [128, 8], F32)
        atmp = sbt(sa, "atmp", [128, 512], F32)
        hm = sbt(sa, "hm", [128, 1024], F32)
        ss8 = sbt(sa, "ss8", [128, 8], F32)
        rs8 = sbt(sa, "rs8", [128, 8], F32)
        cat = sbt(sa, "cat", [128, 1024], BF16)
        catT = sbt(sa, "catT", [128, 8, 128], BF16)
        qkn = sbt(sa, "qkn", [128, 1024], BF16)
        ssq = sbt(sa, "ssq", [128, 16], F32)
        rsq = sbt(sa, "rsq", [128, 16], F32)
        sq = sbt(sa, "sq", [128, 1024], F32)
        qT = sbt(sa, "qT", [128, 4, 128], BF16)
        kc_ = sbt(sa, "kcache", [128, 4, S], BF16)
        vc_ = sbt(sa, "vcache", [128, TPS, 4, 129], BF16)
        pT = [sbt(sa, "pT%d" % i, [128, 512], BF16) for i in range(3)]
        oraw = sbt(sa, "oraw", [128, 4, 2, 129], F32)
        arec = sbt(sa, "arec", [128, 4, 2], F32)
        x1 = sbt(sa, "x1", [128, 1024], F32)
        sx = sbt(sa, "sx", [128, 8], F32)
        h2f = sq
        h2hi = sbt(sa, "h2hi", [128, 1024], BF16)
        h2lo = sbt(sa, "h2lo", [128, 1024], BF16)
        h2T = [sbt(sa, "h2T%d" % i, [128, 8, 128], BF16) for i in range(2)]
        lg = sbt(sa, "lg", [128, 36], F32)
        rt = sbt(sa, "rt", [128, 64], F32)
        rt3 = sbt(sa, "rt3", [128, 4, 8], F32)
        m8 = sbt(sa, "m8", [128, 8], F32)

        tk.dve(lambda e: e.memset(vm[:], 1.0), w=["vm"])
        tk.dve(lambda e: e.memset(vc_[:], 1.0), w=["vcall"])
        tk.barrier()

        def bfv(i):
            return ps[i][:].bitcast(BF16)

        def transposes8(src, dst_fn, srckey, post, n=8):
            b = bank()
            v = bfv(b)
            for k in range(n):
                tk.pe(lambda e, k=k: e.transpose(out=v[:, k * 128:(k + 1) * 128], in_=src[:, k * 128:(k + 1) * 128],
                                                 identity=ident_b[:]),
                      r=(srckey if isinstance(srckey, list) else [srckey]), w=["ps%d" % b])
            post(v, "ps%d" % b)

        def rstd(ss_ap, tmp_ap, out_ap, invd, rk, wk):
            tk.dve(lambda e: e.tensor_scalar(out=tmp_ap, in0=ss_ap, scalar1=invd, scalar2=EPS, op0=ALU.mult,
                                             op1=ALU.add), r=[rk], w=[wk + "_t"])
            tk.act(lambda e: e.activation(out=tmp_ap, in_=tmp_ap, func=AF.Sqrt), r=[wk + "_t"], w=[wk + "_t"])
            tk.dve(lambda e: e.reciprocal(out=out_ap, in_=tmp_ap), r=[wk + "_t"], w=[wk])

        pre = []
        for ex in range(NE):
            pre.append((wbf[ex * 128:(ex + 1) * 128, 0:4096].rearrange("p (kc n) -> p kc n", kc=8),
                        w_gate[0, ex].rearrange("(kc p) n -> p kc n", p=128)))
            pre.append((wbf[ex * 128:(ex + 1) * 128, 4096:8192].rearrange("p (kc n) -> p kc n", kc=8),
                        w_up[0, ex].rearrange("(kc p) n -> p kc n", p=128)))
            pre.append((wbf[ex * 128:(ex + 1) * 128, 8192:12288].rearrange("p (kc n) -> p kc n", kc=4),
                        w_down[0, ex].rearrange("(kc p) n -> p kc n", p=128)))
        pre_i = [0]

        def issue_pre(n):
            for _ in range(n):
                if pre_i[0] < len(pre):
                    o_, i_ = pre[pre_i[0]]
                    tk.dma("pool", lambda e, o_=o_, i_=i_: e.dma_start(out=o_, in_=i_), w=["pre%d" % pre_i[0]])
                    pre_i[0] += 1

        def router_t(ti):
            for i_, (src, sk) in enumerate(((h2hi, "h2hi"), (h2lo, "h2lo"))):
                transposes8(src, None, sk, lambda v, pk, i_=i_: tk.dve(
                    lambda e: e.tensor_copy(out=h2T[i_][:].rearrange("p a b -> p (a b)"), in_=v[:, 0:1024]),
                    r=[pk], w=["h2T%d" % i_]))

        def router_m1(ti):
            bl = bank()
            combos = [(0, wr_hi), (0, wr_lo), (1, wr_hi)]
            n_mm = 0
            for i_, wr in combos:
                for k in range(8):
                    tk.pe(lambda e, i_=i_, wr=wr, k=k, n_mm=n_mm: e.matmul(ps[bl][:, 0:36], lhsT=h2T[i_][:, k, :],
                                                                           rhs=wr[:, k, :], start=(n_mm == 0),
                                                                           stop=(n_mm == 23)),
                          r=["h2T0", "h2T1"], w=["ps%d" % bl])
                    n_mm += 1
            tk.dve(lambda e: e.tensor_tensor(out=lg[:], in0=ps[bl][:, 0:36], in1=rbias[:], op=ALU.add),
                   r=["ps%d" % bl], w=["lg"])
            tk.dve(lambda e: e.tensor_reduce(out=rt[:, 0:1], in_=lg[:, 0:4], axis=AX.X, op=ALU.max), r=["lg"], w=["rt0"])
            tk.dve(lambda e: e.tensor_scalar(out=rt[:, 4:8], in0=lg[:, 0:4], scalar1=rt[:, 0:1], scalar2=None,
                                             op0=ALU.is_equal), r=["lg", "rt0"], w=["gone"])
            tk.dve(lambda e: e.tensor_scalar(out=rt[:, 1:2], in0=rt[:, 0:1], scalar1=-1.0, scalar2=None, op0=ALU.mult),
                   r=["rt0"], w=["rt1"])
            tk.dve(lambda e: e.tensor_tensor(out=rt3[:], in0=lg[:, 4:36].rearrange("p (g x) -> p g x", g=4),
                                             in1=rt[:, 4:8].unsqueeze(2).to_broadcast([128, 4, 8]), op=ALU.mult),
                   r=["lg", "gone"], w=["rt3"])
            tk.dve(lambda e: e.tensor_reduce(out=rt[:, 16:24], in_=rt3[:].rearrange("p g x -> p x g"), axis=AX.X,
                                             op=ALU.add), r=["rt3"], w=["ein"])
            tk.dve(lambda e: e.tensor_tensor(out=rt[:, 12:16], in0=rt[:, 4:8], in1=iota32[:, 0:4], op=ALU.mult),
                   r=["gone"], w=["gi4"])
            tk.dve(lambda e: e.tensor_reduce(out=rt[:, 24:25], in_=rt[:, 12:16], axis=AX.X, op=ALU.add), r=["gi4"], w=["gidx"])
            tk.dve(lambda e: e.max(out=m8[:], in_=rt[:, 16:24]), r=["ein"], w=["m8"])
            for k_, (eid, rw) in enumerate(((eid1, rw1), (eid2, rw2))):
                tk.dve(lambda e, k_=k_: e.tensor_scalar(out=rt[:, 32:40], in0=rt[:, 16:24], scalar1=m8[:, k_:k_ + 1],
                                                        scalar2=None, op0=ALU.is_equal), r=["ein", "m8"], w=["msk"])
                tk.dve(lambda e: e.tensor_tensor(out=rt[:, 32:40], in0=rt[:, 32:40], in1=iota32[:, 0:8], op=ALU.mult),
                       r=["msk"], w=["msk"])
                tk.dve(lambda e: e.tensor_reduce(out=rt[:, 25:26], in_=rt[:, 32:40], axis=AX.X, op=ALU.add),
                       r=["msk"], w=["eidx"])
                tk.dve(lambda e, eid=eid: e.scalar_tensor_tensor(out=eid[:, ti:ti + 1], in0=rt[:, 24:25], scalar=8.0,
                                                                 in1=rt[:, 25:26], op0=ALU.mult, op1=ALU.add),
                       r=["gidx", "eidx"], w=["eid"])
            tk.dve(lambda e: e.tensor_tensor(out=rt[:, 26:27], in0=m8[:, 1:2], in1=m8[:, 0:1], op=ALU.subtract),
                   r=["m8"], w=["dd"])

        def router_m2(ti):
            tk.act(lambda e: e.activation(out=rt[:, 8:12], in_=lg[:, 0:4], func=AF.Exp, bias=rt[:, 1:2],
                                          accum_out=rt[:, 2:3]), r=["lg", "rt1"], w=["rt2"])
            tk.dve(lambda e: e.reciprocal(out=rt[:, 3:4], in_=rt[:, 2:3]), r=["rt2"], w=["gw"])
            tk.act(lambda e: e.activation(out=rt[:, 27:28], in_=rt[:, 26:27], func=AF.Exp), r=["dd"], w=["ex"])
            tk.dve(lambda e: e.tensor_scalar(out=rt[:, 28:29], in0=rt[:, 27:28], scalar1=1.0, scalar2=None, op0=ALU.add),
                   r=["ex"], w=["ex1"])
            tk.dve(lambda e: e.reciprocal(out=rt[:, 29:30], in_=rt[:, 28:29]), r=["ex1"], w=["p1"])
            tk.dve(lambda e: e.tensor_tensor(out=rw1[:, ti:ti + 1], in0=rt[:, 29:30], in1=rt[:, 3:4], op=ALU.mult),
                   r=["p1", "gw"], w=["rw1"])
            tk.dve(lambda e: e.tensor_tensor(out=rw2[:, ti:ti + 1], in0=rw1[:, ti:ti + 1], in1=rt[:, 27:28], op=ALU.mult),
                   r=["rw1", "ex"], w=["rw2"])


        def P1a(ti):
            qt = ti % TPS
            xt = xb[ti % 3]
            xk = "xb%d" % (ti % 3)
            tk.act(lambda e: e.activation(out=junk[:], in_=xt[:], func=AF.Square, accum_out=sx[:, 0:1]),
                   r=[xk], w=["sx0"])
            rstd(sx[:, 0:1], sx[:, 1:2], sx[:, 2:3], 1.0 / 1024, "sx0", "sx2")
            tk.act(lambda e: e.activation(out=hb[:], in_=xt[:], func=AF.Copy, scale=sx[:, 2:3]),
                   r=[xk, "sx2"], w=["hb"])

        def P1b(ti):
            qt = ti % TPS
            xt = xb[ti % 3]
            xk = "xb%d" % (ti % 3)
            transposes8(hb, None, "hb", lambda v, pk: tk.dve(
                lambda e: e.tensor_copy(out=hT[:].rearrange("p a b -> p (a b)"), in_=v[:, 0:1024]), r=[pk], w=["hT"]))

        def P2(ti):
            qt = ti % TPS
            xt = xb[ti % 3]
            xk = "xb%d" % (ti % 3)
            bg = bank()
            for gi, col in enumerate((2048, 2052)):
                for k in range(8):
                    tk.pe(lambda e, gi=gi, col=col, k=k: e.matmul(ps[bg][0:4, gi * 128:(gi + 1) * 128],
                                                                  lhsT=win[:, k, col:col + 4], rhs=hT[:, k, :],
                                                                  start=(k == 0), stop=(k == 7)), r=["hT"], w=["ps%d" % bg])
            if qt == 0:
                tk.dve(lambda e: e.memset(gcar[:, 0:2], 0.0), w=["gcar"])
                tk.dve(lambda e: e.memset(Cst[:], 0.0), w=["Cst"])
            tk.act(lambda e: e.activation(out=gt[:, 0, :], in_=ps[bg][0:4, 128:256], func=AF.Exp, bias=gbias[:, 2:3],
                                          scale=-1.0), r=["ps%d" % bg], w=["gt0"])
            tk.dve(lambda e: e.tensor_scalar(out=gt[:, 0, :], in0=gt[:, 0, :], scalar1=1.0, scalar2=None, op0=ALU.add),
                   r=["gt0"], w=["gt0"])
            tk.act(lambda e: e.activation(out=gt[:, 1, :], in_=gt[:, 0, :], func=AF.Ln), r=["gt0"], w=["gt1"])
            tk.dve(lambda e: e.tensor_tensor_scan(out=gt[:, 2, :], data0=gt[:, 1, :], data1=zeros_f[0:4, :],
                                                  initial=gcar[:, 0:1], op0=ALU.add, op1=ALU.add),
                   r=["gt1", "gcar"], w=["gt2"])
            tk.dve(lambda e: e.scalar_tensor_tensor(out=gt[:, 3, :], in0=ps[bg][0:4, 0:128], scalar=gbias[:, 0:1],
                                                    in1=gt[:, 2, :], op0=ALU.add, op1=ALU.add),
                   r=["ps%d" % bg, "gt2"], w=["gt3"])
            tk.dve(lambda e: e.tensor_reduce(out=gcar[:, 2:3], in_=gt[:, 3, :], axis=AX.X, op=ALU.max),
                   r=["gt3"], w=["gcar2"])
            tk.dve(lambda e: e.tensor_tensor(out=gcar[:, 3:4], in0=gcar[:, 1:2], in1=gcar[:, 2:3], op=ALU.max),
                   r=["gcar", "gcar2"], w=["gcar3"])
            tk.dve(lambda e: e.tensor_scalar(out=gcar[:, 4:5], in0=gcar[:, 3:4], scalar1=-1.0,
                                             scalar2=-0.5 * math.log(128.0), op0=ALU.mult, op1=ALU.add),
                   r=["gcar3"], w=["gcar4"])
            tk.dve(lambda e: e.tensor_scalar(out=gcar[:, 5:6], in0=gcar[:, 3:4], scalar1=-1.0, scalar2=None,
                                             op0=ALU.mult), r=["gcar3"], w=["gcar5"])
            tk.dve(lambda e: e.tensor_tensor(out=gcar[:, 6:7], in0=gcar[:, 1:2], in1=gcar[:, 3:4], op=ALU.subtract),
                   r=["gcar", "gcar3"], w=["gcar6"])
            tk.act(lambda e: e.activation(out=R[:, 0, :], in_=gt[:, 3, :], func=AF.Exp, bias=gcar[:, 4:5]),
                   r=["gt3", "gcar4"], w=["R0"])
            tk.act(lambda e: e.activation(out=R[:, 1, :], in_=gt[:, 2, :], func=AF.Exp, bias=gcar[:, 5:6]),
                   r=["gt2", "gcar5"], w=["R1"])
            tk.act(lambda e: e.activation(out=R[:, 2, :], in_=zeros_f[0:4, :], func=AF.Exp, bias=gcar[:, 6:7]),
                   r=["gcar6"], w=["R2"])
            tk.dve(lambda e: e.tensor_copy(out=gcar[:, 0:1], in_=gt[:, 2, 127:128]), r=["gt2"], w=["gcar"])
            tk.dve(lambda e: e.tensor_copy(out=gcar[:, 1:2], in_=gcar[:, 3:4]), r=["gcar3", "gcar6"], w=["gcar"])
            tk.dve(lambda e: e.tensor_copy(out=Rhi[:], in_=R[:]), r=["R0", "R1", "R2"], w=["Rhi"])
            tk.dve(lambda e: e.tensor_copy(out=Rt[:], in_=Rhi[:]), r=["Rhi"], w=["Rt"])
            tk.dve(lambda e: e.tensor_tensor(out=Rlo[:], in0=R[:], in1=Rt[:], op=ALU.subtract),
                   r=["R0", "R1", "R2", "Rt"], w=["Rlo"])
            if qt == 0:
                tk.dve(lambda e: e.memset(qkpre[:, :, 0:3], 0.0), w=["qkpre"])
            else:
                tk.dve(lambda e: e.tensor_copy(out=qkpre[:, :, 0:3], in_=qkpre[:, :, 128:131]), r=["qkpre"], w=["qkpre"])
            for half in range(2):
                b = bank()
                for cc in range(4):
                    c = half * 4 + cc
                    for k in range(8):
                        tk.pe(lambda e, c=c, cc=cc, k=k, b=b: e.matmul(
                            ps[b][:, cc * 128:(cc + 1) * 128], lhsT=win[:, k, c * 128:(c + 1) * 128], rhs=hT[:, k, :],
                            start=(k == 0), stop=(k == 7)), r=["hT"], w=["ps%d" % b])
                tk.dve(lambda e, half=half, b=b: e.tensor_copy(
                    out=qkpre[:, half * 4:(half + 1) * 4, 3:131],
                    in_=ps[b][:].rearrange("p (c t) -> p c t", c=4)), r=["ps%d" % b], w=["qkpre"])

        def P3a(ti):
            qt = ti % TPS
            xt = xb[ti % 3]
            xk = "xb%d" % (ti % 3)
            bmv, bmo = bank(), bank()
            for b, col in ((bmv, 1024), (bmo, 1536)):
                for k in range(8):
                    tk.pe(lambda e, b=b, col=col, k=k: e.matmul(ps[b][:], lhsT=hT[:, k, :], rhs=win[:, k, col:col + 512],
                                                                 start=(k == 0), stop=(k == 7)), r=["hT"], w=["ps%d" % b])
            tk.dve(lambda e: e.tensor_copy(out=vm[:, :, 0:128], in_=ps[bmv][:].rearrange("p (h d) -> p h d", h=4)),
                   r=["ps%d" % bmv], w=["vm"])
            tk.act(lambda e: e.activation(out=og[:], in_=ps[bmo][:], func=AF.Sigmoid), r=["ps%d" % bmo], w=["og"])
            bq, bk = bank(), bank()
            for b, col in ((bq, 2056), (bk, 2568)):
                for k in range(8):
                    tk.pe(lambda e, b=b, col=col, k=k: e.matmul(ps[b][:], lhsT=hT[:, k, :], rhs=win[:, k, col:col + 512],
                                                                 start=(k == 0), stop=(k == 7)), r=["hT"], w=["ps%d" % b])
            for i_, b in enumerate((bq, bk)):
                tk.act(lambda e, i_=i_, b=b: e.activation(out=sq[:, i_ * 512:(i_ + 1) * 512], in_=ps[b][:], func=AF.Square),
                       r=["ps%d" % b], w=["sq"])
                tk.act(lambda e, i_=i_, b=b: e.activation(out=qkn[:, i_ * 512:(i_ + 1) * 512], in_=ps[b][:], func=AF.Copy),
                       r=["ps%d" % b], w=["qkn"])
            tk.dve(lambda e: e.tensor_reduce(out=ssq[:], in_=sq[:].rearrange("p (g d) -> p g d", d=64), axis=AX.X,
                                             op=ALU.add), r=["sq"], w=["ssq"])
            rstd(ssq[:], rtmp[:], rsq[:], 1.0 / 64, "ssq", "rsq")
            for i_, b in enumerate((bq, bk)):
                tk.dve(lambda e, i_=i_, b=b: e.tensor_tensor(
                    out=qkn[:, i_ * 512:(i_ + 1) * 512].rearrange("p (g d) -> p g d", d=64),
                    in0=qkn[:, i_ * 512:(i_ + 1) * 512].rearrange("p (g d) -> p g d", d=64),
                    in1=rsq[:, i_ * 8:(i_ + 1) * 8].unsqueeze(2).to_broadcast([128, 8, 64]), op=ALU.mult),
                    r=["qkn", "rsq"], w=["qkn"])
            bv = bank()
            for k in range(8):
                tk.pe(lambda e, k=k: e.matmul(ps[bv][:], lhsT=hT[:, k, :], rhs=win[:, k, 3080:3592],
                                              start=(k == 0), stop=(k == 7)), r=["hT"], w=["ps%d" % bv])
            tk.act(lambda e: e.activation(out=vc_[:, qt, :, 0:128], in_=ps[bv][:].rearrange("p (h d) -> p h d", h=4),
                                          func=AF.Copy), r=["ps%d" % bv], w=["vc%d" % qt])

        def P3b1(ti):
            qt = ti % TPS
            xt = xb[ti % 3]
            xk = "xb%d" % (ti % 3)
            for half in range(2):
                b = bank()
                for cc in range(4):
                    c = half * 4 + cc
                    for j in range(4):
                        tk.pe(lambda e, c=c, cc=cc, j=j, b=b: e.matmul(
                            ps[b][:, cc * 128:(cc + 1) * 128], lhsT=convdiag[:, j * 8 + c, :], rhs=qkpre[:, c, j:j + 128],
                            start=(j == 0), stop=(j == 3)), r=["qkpre"], w=["ps%d" % b])
                for cc in range(4):
                    c = half * 4 + cc
                    tk.act(lambda e, c=c, cc=cc, b=b: e.activation(out=qkT[:, c, :], in_=ps[b][:, cc * 128:(cc + 1) * 128],
                                                                   func=AF.Silu, bias=convb[:, c:c + 1]),
                           r=["ps%d" % b], w=["qkT%d" % c])

        def P3b2(ti):
            qt = ti % TPS
            xt = xb[ti % 3]
            xk = "xb%d" % (ti % 3)
            bc = bank()
            for bi in range(3):
                tk.pe(lambda e, bi=bi: e.matmul(ps[bc][:, bi * 4:(bi + 1) * 4], lhsT=Rhi[:, bi, :], rhs=ident_b[0:4, 0:4],
                                                start=True, stop=False), r=["Rhi"], w=["ps%d" % bc])
                tk.pe(lambda e, bi=bi: e.matmul(ps[bc][:, bi * 4:(bi + 1) * 4], lhsT=Rlo[:, bi, :], rhs=ident_b[0:4, 0:4],
                                                start=False, stop=True), r=["Rlo"], w=["ps%d" % bc])
            tk.dve(lambda e: e.tensor_copy(out=gc[:], in_=ps[bc][:, 0:12]), r=["ps%d" % bc], w=["gc"])
            bs = bank()
            for h in range(4):
                tk.pe(lambda e, h=h: e.matmul(ps[bs][:, h * 128:(h + 1) * 128], lhsT=qkT[:, 4 + h, :], rhs=qkT[:, h, :],
                                              start=True, stop=True), r=["qkT%d" % h, "qkT%d" % (4 + h)], w=["ps%d" % bs])
            for h in range(4):
                tk.dve(lambda e, h=h: e.scalar_tensor_tensor(out=sTp[:, h, :], in0=ps[bs][:, h * 128:(h + 1) * 128],
                                                             scalar=gc[:, h:h + 1], in1=tri[:], op0=ALU.mult,
                                                             op1=ALU.mult), r=["ps%d" % bs, "gc"], w=["sTp%d" % h])
            bt = bank()
            vt = bfv(bt)
            for h in range(4):
                tk.pe(lambda e, h=h: e.transpose(out=vt[:, h * 128:(h + 1) * 128], in_=qkT[:, 4 + h, :], identity=ident_b[:]),
                      r=["qkT%d" % (4 + h)], w=["ps%d" % bt])
            for h in range(4):
                tk.act(lambda e, h=h: e.activation(out=kw[:, h, :], in_=vt[:, h * 128:(h + 1) * 128], func=AF.Copy,
                                                   scale=gc[:, h:h + 1]), r=["ps%d" % bt, "gc"], w=["kw%d" % h])
            for h in range(4):
                tk.dve(lambda e, h=h: e.tensor_scalar(out=Cst[:, h, :], in0=Cst[:, h, :], scalar1=gc[:, 8 + h:9 + h],
                                                      scalar2=None, op0=ALU.mult), r=["Cst", "gc"], w=["Cst"])
            tk.dve(lambda e: e.tensor_copy(out=Cbf[:], in_=Cst[:]), r=["Cst"], w=["Cbf"])
            bn = [bank(), bank()]
            for h in range(4):
                o_ = ps[bn[h // 2]][:, (h % 2) * 129:(h % 2) * 129 + 129]
                tk.pe(lambda e, h=h, o_=o_: e.matmul(o_, lhsT=sTp[:, h, :], rhs=vm[:, h, :], start=True, stop=False),
                      r=["sTp%d" % h, "vm"], w=["ps%d" % bn[h // 2]])
                tk.pe(lambda e, h=h, o_=o_: e.matmul(o_, lhsT=qkT[:, h, :], rhs=Cbf[:, h, :], start=False, stop=True),
                      r=["qkT%d" % h, "Cbf"], w=["ps%d" % bn[h // 2]])
            bu = [bank(), bank()]
            for h in range(4):
                o_ = ps[bu[h // 2]][:, (h % 2) * 129:(h % 2) * 129 + 129]
                tk.pe(lambda e, h=h, o_=o_: e.matmul(o_, lhsT=kw[:, h, :], rhs=vm[:, h, :], start=True, stop=True),
                      r=["kw%d" % h, "vm"], w=["ps%d" % bu[h // 2]])
            for h2_ in range(2):
                tk.dve(lambda e, h2_=h2_: e.tensor_tensor(
                    out=Cst[:, 2 * h2_:2 * h2_ + 2, :], in0=Cst[:, 2 * h2_:2 * h2_ + 2, :],
                    in1=ps[bu[h2_]][:, 0:258].rearrange("p (h d) -> p h d", h=2), op=ALU.add),
                    r=["Cst", "ps%d" % bu[h2_], "Cbf"], w=["Cst"])
            for h in range(4):
                nd = ps[bn[h // 2]]
                off = (h % 2) * 129
                pk = "ps%d" % bn[h // 2]
                tk.dve(lambda e, h=h, nd=nd, off=off: e.tensor_copy(out=dtmp[:, h:h + 1], in_=nd[:, off + 128:off + 129]),
                       r=[pk], w=["dt%d" % h])
                tk.dve(lambda e, h=h: e.scalar_tensor_tensor(
                    out=mtmp[:, h:h + 1], in0=dtmp[:, h:h + 1], scalar=-1.0, in1=dtmp[:, h:h + 1],
                    op0=ALU.mult, op1=ALU.max), r=["dt%d" % h], w=["mt%d" % h])
                tk.dve(lambda e, h=h: e.tensor_tensor(out=mtmp[:, h:h + 1], in0=mtmp[:, h:h + 1], in1=gc[:, 4 + h:5 + h],
                                                      op=ALU.max), r=["mt%d" % h, "gc"], w=["mt%d" % h])
                tk.dve(lambda e, h=h: e.reciprocal(out=mtmp[:, 4 + h:5 + h], in_=mtmp[:, h:h + 1]),
                       r=["mt%d" % h], w=["mr%d" % h])
                tk.dve(lambda e, h=h, nd=nd, off=off: e.scalar_tensor_tensor(
                    out=hm[:, h * 128:(h + 1) * 128], in0=nd[:, off:off + 128], scalar=mtmp[:, 4 + h:5 + h],
                    in1=og[:, h * 128:(h + 1) * 128], op0=ALU.mult, op1=ALU.mult),
                    r=[pk, "mr%d" % h, "og"], w=["hm%d" % h])
                tk.act(lambda e, h=h: e.activation(out=junk[:, h * 128:(h + 1) * 128], in_=hm[:, h * 128:(h + 1) * 128],
                                                   func=AF.Square, accum_out=ss8[:, h:h + 1]),
                       r=["hm%d" % h], w=["ss8_%d" % h])
            def post_qk(v, pk):
                tk.dve(lambda e: e.tensor_scalar(out=qT[:].rearrange("p a b -> p (a b)"), in0=v[:, 0:512],
                                                 scalar1=nqk[:, 0:1], scalar2=None, op0=ALU.mult), r=[pk], w=["qT"])
                tk.act(lambda e: e.activation(out=kc_[:, :, qt * 128:(qt + 1) * 128],
                                              in_=v[:, 512:1024].rearrange("p (h t) -> p h t", h=4), func=AF.Copy,
                                              scale=nqk[:, 1:2]), r=[pk], w=["kc%d" % qt])
            transposes8(qkn, None, "qkn", post_qk)

        def P4(ti):
            qt = ti % TPS
            xt = xb[ti % 3]
            xk = "xb%d" % (ti % 3)
            nk = qt + 1
            items = []
            for h in range(4):
                for c in range(2):
                    for g0 in range(0, nk, 4):
                        items.append((h, c, list(range(g0, min(nk, g0 + 4)))))
            pti = [0]

            def qk_exp(it):
                h, c, kts = it
                b = bank()
                p_ = pti[0] % 3
                pti[0] += 1
                for j, kt in enumerate(kts):
                    tk.pe(lambda e, j=j, kt=kt: e.matmul(ps[b][:, j * 128:(j + 1) * 128],
                                                         lhsT=kc_[c * 64:(c + 1) * 64, h, kt * 128:(kt + 1) * 128],
                                                         rhs=qT[c * 64:(c + 1) * 64, h, :], start=True, stop=True),
                          r=["kc%d" % kt, "qT"], w=["ps%d" % b])
                n = len(kts)
                tk.act(lambda e: e.activation(out=pT[p_][:, 0:n * 128], in_=ps[b][:, 0:n * 128], func=AF.Exp, scale=0.125),
                       r=["ps%d" % b], w=["pT%d" % p_])
                if kts[-1] == qt:
                    j = n - 1
                    tk.pool(lambda e: e.memset(pT[p_][64:128, j * 128:j * 128 + 64], 0.0), w=["pT%d" % p_])
                return p_

            def pv(it, p_):
                h, c, kts = it
                pa = 6 + (h % 2)
                for j, kt in enumerate(kts):
                    tk.pe(lambda e, j=j, kt=kt: e.matmul(ps[pa][:, c * 129:(c + 1) * 129], lhsT=pT[p_][:, j * 128:(j + 1) * 128],
                                                         rhs=vc_[:, kt, h, :], start=(kt == 0), stop=(kt == qt)),
                          r=["pT%d" % p_, "vc%d" % kt], w=["ps%d" % pa])
                if c == 1 and kts[-1] == qt:
                    tk.act(lambda e: e.activation(out=oraw[:, h, :, :].rearrange("p a b -> p (a b)"), in_=ps[pa][:, 0:258],
                                                  func=AF.Copy), r=["ps%d" % pa], w=["oraw%d" % h])
                    tk.dve(lambda e: e.reciprocal(out=arec[:, h, :], in_=oraw[:, h, :, 128]), r=["oraw%d" % h], w=["arec%d" % h])
                    tk.dve(lambda e: e.tensor_scalar(out=arec[:, h, 1:2], in0=arec[:, h, 1:2], scalar1=neglam[:, 0:1],
                                                     scalar2=None, op0=ALU.mult), r=["arec%d" % h], w=["arec%d" % h])
                    tk.dve(lambda e: e.tensor_scalar(out=atmp[:, h * 128:(h + 1) * 128], in0=oraw[:, h, 0, 0:128],
                                                     scalar1=arec[:, h, 0:1], scalar2=None, op0=ALU.mult),
                           r=["oraw%d" % h, "arec%d" % h], w=["sqa%d" % h])
                    tk.dve(lambda e: e.scalar_tensor_tensor(out=hm[:, 512 + h * 128:512 + (h + 1) * 128],
                                                            in0=oraw[:, h, 1, 0:128], scalar=arec[:, h, 1:2],
                                                            in1=atmp[:, h * 128:(h + 1) * 128], op0=ALU.mult, op1=ALU.add),
                           r=["oraw%d" % h, "arec%d" % h, "sqa%d" % h], w=["hm%d" % (4 + h)])
                    tk.act(lambda e: e.activation(out=junk[:, 512 + h * 128:512 + (h + 1) * 128],
                                                  in_=hm[:, 512 + h * 128:512 + (h + 1) * 128], func=AF.Square,
                                                  accum_out=ss8[:, 4 + h:5 + h]), r=["hm%d" % (4 + h)],
                           w=["ss8_%d" % (4 + h)])

            pend_pv = []
            for it in items:
                p_ = qk_exp(it)
                pend_pv.append((it, p_))
                if len(pend_pv) > 2:
                    pv(*pend_pv.pop(0))
            for pp in pend_pv:
                pv(*pp)

        def P5(ti):
            qt = ti % TPS
            xt = xb[ti % 3]
            xk = "xb%d" % (ti % 3)
            tk.dve(lambda e: e.tensor_scalar(out=mtmp[:, 8:16], in0=ss8[:], scalar1=1.0 / 128, scalar2=EPS, op0=ALU.mult,
                                             op1=ALU.add), r=["ss8_%d" % i for i in range(8)], w=["mt8"])
            tk.act(lambda e: e.activation(out=mtmp[:, 8:16], in_=mtmp[:, 8:16], func=AF.Sqrt), r=["mt8"], w=["mt8"])
            tk.dve(lambda e: e.reciprocal(out=rs8[:], in_=mtmp[:, 8:16]), r=["mt8"], w=["rs8"])
            tk.dve(lambda e: e.tensor_tensor(out=cat[:].rearrange("p (g d) -> p g d", d=128),
                                             in0=hm[:].rearrange("p (g d) -> p g d", d=128),
                                             in1=rs8[:].unsqueeze(2).to_broadcast([128, 8, 128]), op=ALU.mult),
                   r=["hm%d" % i for i in range(8)] + ["rs8"], w=["cat"])
            transposes8(cat, None, "cat", lambda v, pk: tk.act(
                lambda e: e.activation(out=catT[:].rearrange("p a b -> p (a b)"), in_=v[:, 0:1024], func=AF.Copy),
                r=[pk], w=["catT"]))
            by = [bank(), bank()]
            for half in range(2):
                for k in range(8):
                    tk.pe(lambda e, half=half, k=k: e.matmul(ps[by[half]][:], lhsT=catT[:, k, :],
                                                             rhs=wout[:, k, half * 512:(half + 1) * 512],
                                                             start=(k == 0), stop=(k == 7)), r=["catT"], w=["ps%d" % by[half]])
            for half in range(2):
                tk.dve(lambda e, half=half: e.tensor_tensor(out=x1[:, half * 512:(half + 1) * 512], in0=ps[by[half]][:],
                                                            in1=xt[:, half * 512:(half + 1) * 512], op=ALU.add),
                       r=["ps%d" % by[half], xk], w=["x1_%d" % half])
            tk.dma("sp", lambda e: e.dma_start(out=x1buf[ti * 128:(ti + 1) * 128, :], in_=x1[:]),
                   r=["x1_0", "x1_1"], w=["x1buf%d" % ti])
            tk.act(lambda e: e.activation(out=junk[:], in_=x1[:], func=AF.Square, accum_out=sx[:, 4:5]),
                   r=["x1_0", "x1_1"], w=["sx4"])
            rstd(sx[:, 4:5], sx[:, 5:6], sx[:, 3:4], 1.0 / 1024, "sx4", "sx3")
            tk.dve(lambda e: e.scalar_tensor_tensor(out=h2f[:], in0=x1[:], scalar=sx[:, 3:4], in1=n2w[:], op0=ALU.mult,
                                                    op1=ALU.mult), r=["x1_0", "x1_1", "sx3"], w=["sq"])
            tk.act(lambda e: e.activation(out=h2hi[:], in_=h2f[:], func=AF.Copy), r=["sq"], w=["h2hi"])
            tk.dma("sp", lambda e: e.dma_start(out=h2buf[ti * 128:(ti + 1) * 128, :], in_=h2hi[:]), r=["h2hi"],
                   w=["h2buf%d" % ti])
            tk.dve(lambda e: e.tensor_tensor(out=h2lo[:], in0=h2f[:], in1=h2hi[:], op=ALU.subtract),
                   r=["sq", "h2hi"], w=["h2lo"])

        for t0 in range(min(2, NT)):
            tk.dma("sp", lambda e, t0=t0: e.dma_start(out=xb[t0 % 3][:], in_=x[t0 * 128:(t0 + 1) * 128, :]), w=["xb%d" % (t0 % 3)])
        P1a(0)
        P1b(0)
        P2(0)
        P3a(0)
        for ti in range(NT):
            if ti + 2 < NT:
                tk.dma("sp", lambda e, ti=ti: e.dma_start(out=xb[(ti + 2) % 3][:], in_=x[(ti + 2) * 128:(ti + 3) * 128, :]),
                       w=["xb%d" % ((ti + 2) % 3)])
            issue_pre(2)
            if ti + 1 < NT:
                P1a(ti + 1)
            P3b1(ti)
            if ti > 0:
                router_t(ti - 1)
            if ti + 1 < NT:
                P1b(ti + 1)
            P3b2(ti)
            if ti > 0:
                router_m1(ti - 1)
            P4(ti)
            if ti > 0:
                router_m2(ti - 1)
            if ti + 1 < NT:
                P2(ti + 1)
                P3a(ti + 1)
            P5(ti)
        router_t(NT - 1)
        router_m1(NT - 1)
        router_m2(NT - 1)
        issue_pre(len(pre))
        tk.barrier()

    NCOL = NT * 32
    CH = [(c0, min(NCOL, c0 + 512)) for c0 in range(0, NCOL, 512)]
    assert 2 * len(CH) <= 8
    sb_ = ExitStack()
    dest1 = sbt(sb_, "dest1", [128, NT], I32)
    dest2 = sbt(sb_, "dest2", [128, NT], I32)
    widx = sbt(sb_, "widx", [128, NB], I32)
    with ExitStack() as s2:
        oh1 = sbt(s2, "oh1", [128, NT, 32], F32)
        oh2 = sbt(s2, "oh2", [128, NT, 32], F32)
        comb = sbt(s2, "comb", [128, NT, 32], BF16)
        base = sbt(s2, "base", [128, NT, 32], F32)
        cumA = sbt(s2, "cumA", [128, NT, 32], F32)
        cumB = sbt(s2, "cumB", [128, NT, 32], F32)
        tot = sbt(s2, "tot", [128, NT, 32], F32)
        cnt = sbt(s2, "cnt", [128, 32], F32)
        cmpb = sbt(s2, "cmpb", [128, 32, MAXB], F32)
        thr = sbt(s2, "thr", [128, MAXB], F32)
        nblk = sbt(s2, "nblk", [128, 32], F32)
        pend = sbt(s2, "pend", [128, 32], F32)
        pstart = sbt(s2, "pstart", [128, 32], F32)
        jB = sbt(s2, "jB", [128, NB], F32)
        cmpe = sbt(s2, "cmpe", [128, NB, 32], F32)
        bef = sbt(s2, "bef", [128, NB], F32)
        dstf = sbt(s2, "dstf", [128, 2, NT], F32)
        tokf = sbt(s2, "tokf", [128, NT], F32)
        toki = sbt(s2, "toki", [128, NT, 16], I32)
        initi = sbt(s2, "initi", [128, NG * 16], I32)
        iot = iota32[:].unsqueeze(1).to_broadcast([128, NT, 32])
        tk.dve(lambda e: e.tensor_tensor(out=oh1[:], in0=eid1[:].unsqueeze(2).to_broadcast([128, NT, 32]), in1=iot,
                                         op=ALU.is_equal), w=["oh1"])
        tk.dve(lambda e: e.tensor_tensor(out=oh2[:], in0=eid2[:].unsqueeze(2).to_broadcast([128, NT, 32]), in1=iot,
                                         op=ALU.is_equal), w=["oh2"])
        tk.dve(lambda e: e.tensor_tensor(out=comb[:], in0=oh1[:], in1=oh2[:], op=ALU.add), r=["oh1", "oh2"], w=["comb"])
        combf = comb[:].rearrange("p a b -> p (a b)")
        for ci, (c0, c1) in enumerate(CH):
            tk.pe(lambda e, ci=ci, c0=c0, c1=c1: e.matmul(ps[ci][:, 0:c1 - c0], lhsT=ustrict[:], rhs=combf[:, c0:c1],
                                                          start=True, stop=True), r=["comb"], w=["ps%d" % ci])
            tk.pe(lambda e, ci=ci, c0=c0, c1=c1: e.matmul(ps[4 + ci][:, 0:c1 - c0], lhsT=ones_b[:], rhs=combf[:, c0:c1],
                                                          start=True, stop=True), r=["comb"], w=["ps%d" % (4 + ci)])
        basef = base[:].rearrange("p a b -> p (a b)")
        totf = tot[:].rearrange("p a b -> p (a b)")
        for ci, (c0, c1) in enumerate(CH):
            tk.dve(lambda e, ci=ci, c0=c0, c1=c1: e.tensor_copy(out=basef[:, c0:c1], in_=ps[ci][:, 0:c1 - c0]),
                   r=["ps%d" % ci], w=["base"])
            tk.act(lambda e, ci=ci, c0=c0, c1=c1: e.activation(out=totf[:, c0:c1], in_=ps[4 + ci][:, 0:c1 - c0],
                                                               func=AF.Copy), r=["ps%d" % (4 + ci)], w=["tot"])
        src, dst = tot, cumA
        d = 1
        while d < NT:
            tk.dve(lambda e, src=src, dst=dst, d=d: e.tensor_copy(out=dst[:, 0:d, :], in_=src[:, 0:d, :]),
                   r=["tot", "cumA", "cumB"], w=["cumA" if dst is cumA else "cumB"])
            tk.dve(lambda e, src=src, dst=dst, d=d: e.tensor_tensor(out=dst[:, d:NT, :], in0=src[:, d:NT, :],
                                                                    in1=src[:, 0:NT - d, :], op=ALU.add),
                   r=["tot", "cumA", "cumB"], w=["cumA" if dst is cumA else "cumB"])
            src = dst
            dst = cumB if dst is cumA else cumA
            d *= 2
        inc = src
        inck = "tot" if inc is tot else ("cumA" if inc is cumA else "cumB")
        tk.dve(lambda e: e.tensor_copy(out=cnt[:], in_=inc[:, NT - 1, :]), r=[inck], w=["cnt"])
        tk.dve(lambda e: e.tensor_tensor(out=base[:], in0=base[:], in1=inc[:], op=ALU.add), r=["base", inck], w=["base"])
        tk.dve(lambda e: e.tensor_tensor(out=base[:], in0=base[:], in1=tot[:], op=ALU.subtract), r=["base", "tot"], w=["base"])
        tk.pool(lambda e: e.iota(thr[:], pattern=[[B, MAXB]], base=0, channel_multiplier=0,
                                 allow_small_or_imprecise_dtypes=True), w=["thr"])
        tk.pool(lambda e: e.iota(jB[:], pattern=[[B, NB]], base=0, channel_multiplier=0,
                                 allow_small_or_imprecise_dtypes=True), w=["jB"])
        tk.pool(lambda e: e.iota(tokf[:], pattern=[[128, NT]], base=0, channel_multiplier=1,
                                 allow_small_or_imprecise_dtypes=True), w=["tokf"])
        tk.dve(lambda e: e.tensor_tensor(out=cmpb[:], in0=cnt[:].unsqueeze(2).to_broadcast([128, 32, MAXB]),
                                         in1=thr[:].unsqueeze(1).to_broadcast([128, 32, MAXB]), op=ALU.is_gt),
               r=["cnt", "thr"], w=["cmpb"])
        tk.dve(lambda e: e.tensor_reduce(out=nblk[:], in_=cmpb[:], axis=AX.X, op=ALU.add), r=["cmpb"], w=["nblk"])
        tk.dve(lambda e: e.tensor_scalar(out=nblk[:], in0=nblk[:], scalar1=float(B), scalar2=None, op0=ALU.mult),
               r=["nblk"], w=["nblk"])
        tk.dve(lambda e: e.tensor_tensor_scan(out=pend[:], data0=nblk[:], data1=zeros_f[:, 0:32], initial=0.0,
                                              op0=ALU.add, op1=ALU.add), r=["nblk"], w=["pend"])
        tk.dve(lambda e: e.tensor_tensor(out=pstart[:], in0=pend[:], in1=nblk[:], op=ALU.subtract),
               r=["pend", "nblk"], w=["pstart"])
        tk.dve(lambda e: e.tensor_tensor(out=base[:], in0=base[:], in1=pstart[:].unsqueeze(1).to_broadcast([128, NT, 32]),
                                         op=ALU.add), r=["base", "pstart"], w=["base"])
        for k_, (oh, dd) in enumerate(((oh1, dest1), (oh2, dest2))):
            tk.dve(lambda e, oh=oh: e.tensor_tensor(out=oh[:], in0=oh[:], in1=base[:], op=ALU.mult),
                   r=["base", "oh1", "oh2", "comb"], w=["oh%d" % (k_ + 1)])
            tk.dve(lambda e, oh=oh, k_=k_: e.tensor_reduce(out=dstf[:, k_, :], in_=oh[:], axis=AX.X, op=ALU.add),
                   r=["oh%d" % (k_ + 1)], w=["dstf%d" % k_])
            tk.dve(lambda e, dd=dd, k_=k_: e.tensor_copy(out=dd[:], in_=dstf[:, k_, :]), r=["dstf%d" % k_], w=["dest%d" % k_])
        tk.dve(lambda e: e.tensor_tensor(out=cmpe[:], in0=pend[:].unsqueeze(1).to_broadcast([128, NB, 32]),
                                         in1=jB[:].unsqueeze(2).to_broadcast([128, NB, 32]), op=ALU.is_le),
               r=["pend", "jB"], w=["cmpe"])
        tk.dve(lambda e: e.tensor_reduce(out=bef[:], in_=cmpe[:], axis=AX.X, op=ALU.add), r=["cmpe"], w=["bef"])
        tk.dve(lambda e: e.tensor_scalar(out=bef[:], in0=bef[:], scalar1=float(NE - 1), scalar2=128.0, op0=ALU.min,
                                         op1=ALU.mult), r=["bef"], w=["bef"])
        if dbg:
            tk.dma("sp", lambda e: e.dma_start(out=dbg_be, in_=bef[:]), r=["bef"], w=["dbg_be"])
            tk.dma("sp", lambda e: e.dma_start(out=dbg_d, in_=dstf[:].rearrange("p a b -> p (a b)")),
                   r=["dstf0", "dstf1"], w=["dbg_d"])
            tk.dma("sp", lambda e: e.dma_start(out=dbg_r[:, 0:NT], in_=eid1[:]), w=["dbg_r0"])
            tk.dma("sp", lambda e: e.dma_start(out=dbg_r[:, NT:2 * NT], in_=eid2[:]), w=["dbg_r1"])
            tk.dma("sp", lambda e: e.dma_start(out=dbg_r[:, 2 * NT:3 * NT], in_=rw1[:]), w=["dbg_r2"])
            tk.dma("sp", lambda e: e.dma_start(out=dbg_r[:, 3 * NT:4 * NT], in_=rw2[:]), w=["dbg_r3"])
        tk.dve(lambda e: e.tensor_scalar(out=bef[:], in0=bef[:], scalar1=pidx[:, 0:1], scalar2=None, op0=ALU.add),
               r=["bef", "dbg_be"], w=["bef"])
        tk.dve(lambda e: e.tensor_copy(out=widx[:], in_=bef[:]), r=["bef"], w=["widx"])
        tk.dve(lambda e: e.memset(toki[:], 0), w=["toki"])
        tk.dve(lambda e: e.tensor_copy(out=toki[:, :, 0], in_=tokf[:]), r=["tokf", "toki"], w=["toki"])
        tk.dve(lambda e: e.memset(initi[:], T), w=["initi"])
        tk.dma("sp", lambda e: e.dma_start(out=slot_tok.rearrange("(p g) c -> p (g c)", p=128), in_=initi[:]),
               r=["initi"], w=["slot_tok"])
        for ti in range(NT):
            for dd, dk in ((dest1, "dest0"), (dest2, "dest1")):
                tk.dma("pool", lambda e, ti=ti, dd=dd: e.indirect_dma_start(
                    out=slot_tok, out_offset=bass.IndirectOffsetOnAxis(ap=dd[:, ti:ti + 1], axis=0),
                    in_=toki[:, ti, :], in_offset=None, bounds_check=bnds["slot"], oob_is_err=False),
                    r=[dk, "toki", "slot_tok"], w=["st_%d_%s" % (ti, dk)])
        tk.barrier()

    with ExitStack() as s3:
        wblk = [sbt(s3, "wblk%d" % i, [128, 12288], BF16) for i in range(2)]
        gidx_all = sbt(s3, "gidx_all", [128, NG], I32)
        for g in range(NG):
            tk.dma("sp", lambda e, g=g: e.dma_start(out=gidx_all[:, g:g + 1], in_=slot_tok[g * 128:(g + 1) * 128, 0:1],
                                                    allow_slow_non_contiguous=True), w=["gidx%d" % g])
        xg = [sbt(s3, "xg%d" % i, [128, 1024], BF16) for i in range(4)]
        xT = [sbt(s3, "xT%d" % i, [128, 8, B], BF16) for i in range(2)]
        sg = [sbt(s3, "sg%d" % i, [128, B], F32) for i in range(2)]
        hid = [sbt(s3, "hid%d" % i, [128, 4, B], BF16) for i in range(2)]
        yst = [sbt(s3, "yst%d" % i, [128, 1024], F32) for i in range(2)]
        gcount = 0
        ycount = 0
        for j in range(NB):
            wb = wblk[j % 2]
            wk = "wblk%d" % (j % 2)
            tk.dma("pool", lambda e, j=j, wb=wb: e.indirect_dma_start(
                out=wb[:], out_offset=None, in_=wbf, in_offset=bass.IndirectOffsetOnAxis(ap=widx[:, j:j + 1], axis=0),
                bounds_check=bnds["w"], oob_is_err=False), w=[wk])
            xTj = xT[j % 2]
            xTk = "xT%d" % (j % 2)
            for gi in range(GB):
                g = j * GB + gi
                s_ = gcount % 4
                gcount += 1
                tk.dma("pool", lambda e, s_=s_, g=g: e.indirect_dma_start(
                    out=xg[s_][:], out_offset=None, in_=h2buf,
                    in_offset=bass.IndirectOffsetOnAxis(ap=gidx_all[:, g:g + 1], axis=0), bounds_check=bnds["h"],
                    oob_is_err=False), r=["gidx%d" % g], w=["xg%d" % s_])
                b = bank()
                v = ps[b][:].bitcast(BF16)
                for k in range(8):
                    tk.pe(lambda e, k=k, s_=s_: e.transpose(out=v[:, k * 128:(k + 1) * 128],
                                                            in_=xg[s_][:, k * 128:(k + 1) * 128], identity=ident_b[:]),
                          r=["xg%d" % s_], w=["ps%d" % b])
                tk.dve(lambda e, gi=gi, v=v: e.tensor_copy(out=xTj[:, :, gi * 128:(gi + 1) * 128],
                                                           in_=v[:, 0:1024].rearrange("p (k t) -> p k t", k=8)),
                       r=["ps%d" % b], w=[xTk + "_%d" % gi])
            hj = hid[j % 2]
            hk = "hid%d" % (j % 2)
            for hc in range(4):
                b = bank()
                for m in range(2):
                    for k in range(8):
                        tk.pe(lambda e, m=m, k=k, hc=hc, b=b: e.matmul(
                            ps[b][:, m * B:(m + 1) * B], lhsT=wb[:, m * 4096 + k * 512 + hc * 128:m * 4096 + k * 512 + hc * 128 + 128],
                            rhs=xTj[:, k, :], start=(k == 0), stop=(k == 7)),
                            r=[wk] + [xTk + "_%d" % gi for gi in range(GB)], w=["ps%d" % b])
                sgi = sg[hc % 2]
                tk.act(lambda e, b=b, sgi=sgi: e.activation(out=sgi[:], in_=ps[b][:, 0:B], func=AF.Silu),
                       r=["ps%d" % b], w=["sg%d" % (hc % 2)])
                tk.dve(lambda e, b=b, sgi=sgi, hc=hc: e.tensor_tensor(out=hj[:, hc, :], in0=sgi[:], in1=ps[b][:, B:2 * B],
                                                                      op=ALU.mult),
                       r=["ps%d" % b, "sg%d" % (hc % 2)], w=[hk + "_%d" % hc])
            for gi in range(GB):
                ys = yst[ycount % 2]
                yk = "yst%d" % (ycount % 2)
                ycount += 1
                for half in range(2):
                    b = bank()
                    for hc in range(4):
                        tk.pe(lambda e, hc=hc, half=half, b=b, gi=gi: e.matmul(
                            ps[b][:], lhsT=hj[:, hc, gi * 128:(gi + 1) * 128],
                            rhs=wb[:, 8192 + hc * 1024 + half * 512:8192 + hc * 1024 + half * 512 + 512],
                            start=(hc == 0), stop=(hc == 3)), r=[wk] + [hk + "_%d" % i for i in range(4)], w=["ps%d" % b])
                    if half == 0:
                        tk.act(lambda e, b=b, ys=ys: e.activation(out=ys[:, 0:512], in_=ps[b][:], func=AF.Copy),
                               r=["ps%d" % b], w=[yk + "a"])
                    else:
                        tk.dve(lambda e, b=b, ys=ys: e.tensor_copy(out=ys[:, 512:1024], in_=ps[b][:]),
                               r=["ps%d" % b], w=[yk + "b"])
                row0 = (j * GB + gi) * 128
                tk.dma("sp", lambda e, ys=ys, row0=row0: e.dma_start(out=ybuf[row0:row0 + 128, :], in_=ys[:]),
                       r=[yk + "a", yk + "b"], w=["ybuf%d" % row0])
        tk.barrier()

    with ExitStack() as s4:
        x1t = [sbt(s4, "x1t%d" % i, [128, 1024], F32) for i in range(2)]
        y1t = [sbt(s4, "y1t%d" % i, [128, 1024], F32) for i in range(2)]
        y2t = [sbt(s4, "y2t%d" % i, [128, 1024], F32) for i in range(2)]
        tk.dma("sp", lambda e: e.dma_start(out=x1t[0][:], in_=x1buf[0:128, :]), w=["x1t0"])
        for ti in range(NT):
            s_ = ti % 2
            if ti + 1 < NT:
                tk.dma("sp", lambda e, ti=ti: e.dma_start(out=x1t[(ti + 1) % 2][:], in_=x1buf[(ti + 1) * 128:(ti + 2) * 128, :]),
                       w=["x1t%d" % ((ti + 1) % 2)])
            for yt, dd, nm in ((y1t, dest1, "y1t"), (y2t, dest2, "y2t")):
                tk.dma("pool", lambda e, ti=ti, s_=s_, yt=yt, dd=dd: e.indirect_dma_start(
                    out=yt[s_][:], out_offset=None, in_=ybuf,
                    in_offset=bass.IndirectOffsetOnAxis(ap=dd[:, ti:ti + 1], axis=0), bounds_check=bnds["slot"],
                    oob_is_err=False), w=["%s%d" % (nm, s_)])
            tk.dve(lambda e, ti=ti, s_=s_: e.scalar_tensor_tensor(out=x1t[s_][:], in0=y1t[s_][:], scalar=rw1[:, ti:ti + 1],
                                                                  in1=x1t[s_][:], op0=ALU.mult, op1=ALU.add),
                   r=["y1t%d" % s_, "x1t%d" % s_], w=["x1t%d" % s_])
            tk.dve(lambda e, ti=ti, s_=s_: e.scalar_tensor_tensor(out=x1t[s_][:], in0=y2t[s_][:], scalar=rw2[:, ti:ti + 1],
                                                                  in1=x1t[s_][:], op0=ALU.mult, op1=ALU.add),
                   r=["y2t%d" % s_, "x1t%d" % s_], w=["x1t%d" % s_])
            tk.dma("sp", lambda e, ti=ti, s_=s_: e.dma_start(out=out[ti * 128:(ti + 1) * 128, :], in_=x1t[s_][:]),
                   r=["x1t%d" % s_], w=["out%d" % ti])
        tk.barrier()
    sb_.close()
    cs.close()
    return nc, tk


PARAMS = ["norm1_w", "w_in", "b_igate", "b_fgate", "conv_w", "conv_b", "m_norm_w", "q_norm_w", "k_norm_w",
          "lambda_q1", "lambda_k1", "lambda_q2", "lambda_k2", "a_norm_w", "w_out", "norm2_w", "w_group", "b_group",
          "w_expert", "b_expert", "w_gate", "w_up", "w_down"]


def run(inputs, n_cores, NSEQ, S, dbg=False, trace=False):
    nc, tk = build(NSEQ=NSEQ, S=S, dbg=dbg)
    xs = np.ascontiguousarray(inputs["x"], dtype=np.float32).reshape(n_cores, NSEQ * S, 1024)
    shared = {k: np.ascontiguousarray(inputs[k], dtype=np.float32) for k in PARAMS}
    in_maps = []
    for c in range(n_cores):
        m = dict(shared)
        m["x"] = xs[c]
        in_maps.append(m)
    res = run_bass_kernel_spmd(nc, in_maps, core_ids=list(range(n_cores)), **({"trace": True} if trace else {}))
    return res


def kernel(**inputs):
    res = run(inputs, 8, 4, 2048)
    outs = [np.asarray(r["out"]).reshape(4, 2048, 1024) for r in res.results]
    return np.concatenate(outs, axis=0).astype(np.float32)
```

```python
import math
import os
from contextlib import ExitStack
import numpy as np
import concourse.bass as bass
import concourse.mybir as mybir
from concourse.bass_utils import run_bass_kernel_spmd

F32 = mybir.dt.float32
BF16 = mybir.dt.bfloat16
I32 = mybir.dt.int32
ALU = mybir.AluOpType
AF = mybir.ActivationFunctionType
AX = mybir.AxisListType
EPS = 1e-6
BLK = 256


class Trk:
    EPOCH = int(os.environ.get("TRK_EPOCH", "30000"))

    def __init__(self, nc):
        self.nc = nc
        self.E = dict(pe=nc.tensor, act=nc.scalar, dve=nc.vector, pool=nc.gpsimd, sp=nc.sync)
        self.S = []
        self.sem = {}
        self.cnt = {}
        self.final = {}
        for e in self.E:
            self._newsem(e)
        self.waited = {}
        self.lastw = {}
        self.readers = {}
        self.dsems = {q: [self._alloc("d%s%d" % (q, i)) for i in range(8)] for q in ("sp", "act", "pool")}
        self.dcnt = {}
        self.drr = {q: 0 for q in self.dsems}
        self.ninst = 0

    def _alloc(self, name):
        s = self.nc.semaphore(name).__enter__()
        self.S.append(s)
        return len(self.S) - 1

    def _newsem(self, e):
        if e in self.sem:
            self.final[self.sem[e]] = (self.cnt[e], e)
        self.sem[e] = self._alloc("e%s%d" % (e, len(self.S)))
        self.cnt[e] = 0

    def _wait(self, eng, tk, force=False):
        si, val, peng = tk
        if peng == eng == "pe" and not force:
            return
        if self.waited.get((eng, si), 0) >= val:
            return
        self.waited[(eng, si)] = val
        self.E[eng].wait_ge(self.S[si], val)
        self.ninst += 1

    def _deps(self, eng, r, w):
        for k in r:
            t = self.lastw.get(k)
            if t:
                self._wait(eng, t)
        for k in w:
            t = self.lastw.get(k)
            if t:
                self._wait(eng, t)
            for t in self.readers.get(k, {}).values():
                self._wait(eng, t)

    def _post(self, tk, r, w):
        for k in w:
            self.lastw[k] = tk
            self.readers[k] = {}
        for k in r:
            self.readers.setdefault(k, {})[tk[0]] = tk

    def op(self, eng, fn, r=(), w=()):
        self._deps(eng, r, w)
        inst = fn(self.E[eng])
        si = self.sem[eng]
        self.cnt[eng] += 1
        val = self.cnt[eng]
        inst.then_inc(self.S[si], 1)
        self.ninst += 1
        tk = (si, val, eng)
        self._post(tk, r, w)
        if val >= self.EPOCH:
            self._newsem(eng)
        return tk

    def pe(self, fn, r=(), w=()):
        return self.op("pe", fn, r, w)

    def act(self, fn, r=(), w=()):
        return self.op("act", fn, r, w)

    def dve(self, fn, r=(), w=()):
        return self.op("dve", fn, r, w)

    def pool(self, fn, r=(), w=()):
        return self.op("pool", fn, r, w)

    def dma(self, q, fn, r=(), w=()):
        self._deps(q, r, w)
        lst = self.dsems[q]
        i = self.drr[q]
        self.drr[q] = (i + 1) % len(lst)
        si = lst[i]
        prev = self.dcnt.get(si, 0)
        if prev:
            self._wait(q, (si, prev, "dma"))
        inst = fn(self.E[q])
        self.dcnt[si] = prev + 16
        inst.then_inc(self.S[si], 16)
        self.ninst += 1
        tk = (si, prev + 16, "dma")
        self._post(tk, r, w)
        return tk

    def barrier(self, engines=None):
        latest = []
        for si, (c, e) in self.final.items():
            if c > 0:
                latest.append((si, c, e))
        for e in self.E:
            if self.cnt[e] > 0:
                latest.append((self.sem[e], self.cnt[e], e))
        for si, c in self.dcnt.items():
            if c > 0:
                latest.append((si, c, "dma"))
        for eng in (engines or self.E):
            for t in latest:
                self._wait(eng, t, force=True)
        self.lastw.clear()
        self.readers.clear()


def build(NSEQ=4, S=2048, dbg=False):
    T = NSEQ * S
    NT = T // 128
    TPS = S // 128
    A = 2 * T
    B = BLK
    MAXB = A // B
    NB = A // B + 32
    NSLOT = NB * B
    NG = NSLOT // 128
    GB = B // 128
    NE = 32
    LAM_INIT = 0.8 - 0.6 * math.exp(-0.3 * 0)

    nc = bass.Bass("TRN2", target_bir_lowering=False)

    def din(name, shape, dt=F32):
        return nc.dram_tensor(name, shape, dt, kind="ExternalInput").ap()

    x = din("x", [T, 1024])
    norm1_w = din("norm1_w", [1, 1024])
    w_in = din("w_in", [1, 1024, 3592])
    b_igate = din("b_igate", [1, 4])
    b_fgate = din("b_fgate", [1, 4])
    conv_w = din("conv_w", [1, 4, 1024])
    conv_b = din("conv_b", [1, 1024])
    m_norm_w = din("m_norm_w", [1, 128])
    q_norm_w = din("q_norm_w", [1, 64])
    k_norm_w = din("k_norm_w", [1, 64])
    lq1 = din("lambda_q1", [1, 64])
    lk1 = din("lambda_k1", [1, 64])
    lq2 = din("lambda_q2", [1, 64])
    lk2 = din("lambda_k2", [1, 64])
    a_norm_w = din("a_norm_w", [1, 128])
    w_out = din("w_out", [1, 1024, 1024])
    norm2_w = din("norm2_w", [1, 1024])
    w_group = din("w_group", [1, 1024, 4])
    b_group = din("b_group", [1, 4])
    w_expert = din("w_expert", [1, 1024, 32])
    b_expert = din("b_expert", [1, 32])
    w_gate = din("w_gate", [1, 32, 1024, 512])
    w_up = din("w_up", [1, 32, 1024, 512])
    w_down = din("w_down", [1, 32, 512, 1024])
    out = nc.dram_tensor("out", [T, 1024], F32, kind="ExternalOutput").ap()
    kd = "ExternalOutput" if dbg else "Internal"
    x1buf = nc.dram_tensor("x1buf", [T, 1024], F32, kind=kd).ap()
    h2buf = nc.dram_tensor("h2buf", [T + 128, 1024], BF16, kind=kd).ap()
    wbf = nc.dram_tensor("wbf", [NE * 128, 12288], BF16, kind="Internal").ap()
    slot_tok = nc.dram_tensor("slot_tok", [NSLOT, 16], I32, kind=kd).ap()
    ybuf = nc.dram_tensor("ybuf", [NSLOT, 1024], F32, kind=kd).ap()
    if dbg:
        dbg_r = nc.dram_tensor("dbg_r", [128, NT * 4], F32, kind="ExternalOutput").ap()
        dbg_d = nc.dram_tensor("dbg_d", [128, NT * 2], F32, kind="ExternalOutput").ap()
        dbg_be = nc.dram_tensor("dbg_be", [128, NB], F32, kind="ExternalOutput").ap()

    tk = Trk(nc)
    bnds = {}
    for nm, val in (("slot", NSLOT - 1), ("w", NE * 128 - 1), ("h", T + 127)):
        rg = nc.gpsimd.register("bnd_" + nm).__enter__()
        nc.gpsimd.reg_mov(rg, val)
        bnds[nm] = nc.gpsimd.snap(rg)
    ps = [nc.psum_tensor("ps%d" % i, [128, 512], F32).__enter__() for i in range(8)]
    rot = [0]

    def bank():
        i = rot[0]
        rot[0] = (i + 1) % 6
        return i

    def sbt(stack, name, shape, dt):
        return stack.enter_context(nc.sbuf_tensor(name, shape, dt))

    cs = ExitStack()
    ident_f = sbt(cs, "ident_f", [128, 128], F32)
    ident_b = sbt(cs, "ident_b", [128, 128], BF16)
    tri = sbt(cs, "tri", [128, 128], F32)
    ustrict = sbt(cs, "ustrict", [128, 128], BF16)
    ones_b = sbt(cs, "ones_b", [128, 128], BF16)
    zeros_f = sbt(cs, "zeros_f", [128, 128], F32)
    iota32 = sbt(cs, "iota32", [128, 32], F32)
    pidx = sbt(cs, "pidx", [128, 1], F32)
    n2w = sbt(cs, "n2w", [128, 1024], F32)
    wr_hi = sbt(cs, "wr_hi", [128, 8, 36], BF16)
    wr_lo = sbt(cs, "wr_lo", [128, 8, 36], BF16)
    rbias = sbt(cs, "rbias", [128, 36], F32)
    eid1 = sbt(cs, "eid1", [128, NT], F32)
    eid2 = sbt(cs, "eid2", [128, NT], F32)
    rw1 = sbt(cs, "rw1", [128, NT], F32)
    rw2 = sbt(cs, "rw2", [128, NT], F32)

    with ExitStack() as s0:
        colf = sbt(s0, "colf", [128, 128], F32)
        rowf = sbt(s0, "rowf", [128, 128], F32)
        wr_f = sbt(s0, "wr_f", [128, 8, 36], F32)
        wr_t = sbt(s0, "wr_t", [128, 8, 36], F32)
        tk.pool(lambda e: e.iota(colf[:], pattern=[[1, 128]], base=0, channel_multiplier=0,
                                 allow_small_or_imprecise_dtypes=True), w=["colf"])
        tk.pool(lambda e: e.iota(rowf[:], pattern=[[0, 128]], base=0, channel_multiplier=1,
                                 allow_small_or_imprecise_dtypes=True), w=["rowf"])
        tk.pool(lambda e: e.iota(iota32[:], pattern=[[1, 32]], base=0, channel_multiplier=0,
                                 allow_small_or_imprecise_dtypes=True), w=["c"])
        tk.pool(lambda e: e.iota(pidx[:], pattern=[[0, 1]], base=0, channel_multiplier=1,
                                 allow_small_or_imprecise_dtypes=True), w=["c2"])
        tk.dve(lambda e: e.tensor_tensor(out=ident_f[:], in0=colf[:], in1=rowf[:], op=ALU.is_equal),
               r=["colf", "rowf"], w=["ident_f"])
        tk.dve(lambda e: e.tensor_copy(out=ident_b[:], in_=ident_f[:]), r=["ident_f"], w=["ident_b"])
        tk.dve(lambda e: e.tensor_tensor(out=tri[:], in0=colf[:], in1=rowf[:], op=ALU.is_ge),
               r=["colf", "rowf"], w=["tri"])
        tk.dve(lambda e: e.tensor_tensor(out=ustrict[:], in0=colf[:], in1=rowf[:], op=ALU.is_gt),
               r=["colf", "rowf"], w=["ustrict"])
        tk.dve(lambda e: e.memset(ones_b[:], 1.0), w=["ones_b"])
        tk.dve(lambda e: e.memset(zeros_f[:], 0.0), w=["zeros_f"])
        tk.dma("sp", lambda e: e.dma_start(out=n2w[:], in_=norm2_w.partition_broadcast(128)), w=["n2w"])
        tk.dma("sp", lambda e: e.dma_start(out=rbias[:, 0:4], in_=b_group.partition_broadcast(128)), w=["rb0"])
        tk.dma("sp", lambda e: e.dma_start(out=rbias[:, 4:36], in_=b_expert.partition_broadcast(128)), w=["rb1"])
        tk.dma("sp", lambda e: e.dma_start(out=wr_f[:, :, 0:4],
                                           in_=w_group[0].rearrange("(kc p) g -> p kc g", p=128)), w=["wrf0"])
        tk.dma("sp", lambda e: e.dma_start(out=wr_f[:, :, 4:36],
                                           in_=w_expert[0].rearrange("(kc p) g -> p kc g", p=128)), w=["wrf1"])
        tk.dve(lambda e: e.tensor_copy(out=wr_hi[:], in_=wr_f[:]), r=["wrf0", "wrf1"], w=["wr_hi"])
        tk.dve(lambda e: e.tensor_copy(out=wr_t[:], in_=wr_hi[:]), r=["wr_hi"], w=["wr_t"])
        tk.dve(lambda e: e.tensor_tensor(out=wr_lo[:], in0=wr_f[:], in1=wr_t[:], op=ALU.subtract),
               r=["wr_t", "wrf0", "wrf1"], w=["wr_lo"])
        tk.barrier()

    with ExitStack() as sz:
        zrow = sbt(sz, "zrow", [128, 1024], BF16)
        tk.dve(lambda e: e.memset(zrow[:], 0.0), w=["zrow"])
        tk.dma("sp", lambda e: e.dma_start(out=h2buf[T:T + 128, :], in_=zrow[:]), r=["zrow"], w=["h2z"])
        tk.barrier()

    with ExitStack() as sa:
        win = sbt(sa, "win", [128, 8, 3592], BF16)
        wout = sbt(sa, "wout", [128, 8, 1024], BF16)
        convdiag = sbt(sa, "convdiag", [128, 32, 128], BF16)
        convb = sbt(sa, "convb", [128, 8], F32)
        nqk = sbt(sa, "nqk", [128, 2], F32)
        gbias = sbt(sa, "gbias", [4, 4], F32)
        neglam = sbt(sa, "neglam", [128, 1], F32)
        with ExitStack() as s1:
            stg = [sbt(s1, "stg%d" % i, [128, 3592], F32) for i in range(2)]
            n1 = sbt(s1, "n1", [128, 8], F32)
            cw = sbt(s1, "cw", [128, 4, 8], F32)
            mnw = sbt(s1, "mnw", [128, 1], F32)
            anw = sbt(s1, "anw", [128, 1], F32)
            lam4 = sbt(s1, "lam4", [128, 4, 64], F32)
            lamp = sbt(s1, "lamp", [128, 2, 64], F32)
            lams = sbt(s1, "lams", [128, 4], F32)
            tk.dma("sp", lambda e: e.dma_start(out=n1[:], in_=norm1_w.rearrange("o (kc p) -> p (o kc)", p=128),
                                               allow_slow_non_contiguous=True), w=["n1"])
            tk.dma("sp", lambda e: e.dma_start(out=cw[:], in_=conv_w[0].rearrange("j (c p) -> p j c", p=128),
                                               allow_slow_non_contiguous=True), w=["cw"])
            tk.dma("sp", lambda e: e.dma_start(out=convb[:], in_=conv_b.rearrange("o (c p) -> p (o c)", p=128),
                                               allow_slow_non_contiguous=True), w=["convb"])
            tk.dma("sp", lambda e: e.dma_start(out=mnw[:], in_=m_norm_w.rearrange("o p -> p o")), w=["mnw"])
            tk.dma("sp", lambda e: e.dma_start(out=anw[:], in_=a_norm_w.rearrange("o p -> p o")), w=["anw"])
            tk.dma("sp", lambda e: e.dma_start(out=nqk[0:64, 0:1], in_=q_norm_w.rearrange("o p -> p o")), w=["nqk0"])
            tk.dma("sp", lambda e: e.dma_start(out=nqk[64:128, 0:1], in_=q_norm_w.rearrange("o p -> p o")), w=["nqk1"])
            tk.dma("sp", lambda e: e.dma_start(out=nqk[0:64, 1:2], in_=k_norm_w.rearrange("o p -> p o")), w=["nqk2"])
            tk.dma("sp", lambda e: e.dma_start(out=nqk[64:128, 1:2], in_=k_norm_w.rearrange("o p -> p o")), w=["nqk3"])
            tk.dma("sp", lambda e: e.dma_start(out=gbias[:, 0:1], in_=b_igate.rearrange("o p -> p o")), w=["gb0"])
            tk.dma("sp", lambda e: e.dma_start(out=gbias[:, 1:2], in_=b_fgate.rearrange("o p -> p o")), w=["gb1"])
            for i, l in enumerate((lq1, lk1, lq2, lk2)):
                tk.dma("sp", lambda e, i=i, l=l: e.dma_start(out=lam4[:, i, :], in_=l.partition_broadcast(128)),
                       w=["lam4_%d" % i])
            tk.dve(lambda e: e.tensor_scalar(out=gbias[:, 2:3], in0=gbias[:, 1:2], scalar1=-1.0, scalar2=None,
                                             op0=ALU.mult), r=["gb1"], w=["gb2"])
            tk.dve(lambda e: e.tensor_scalar(out=anw[:], in0=anw[:], scalar1=1.0 - LAM_INIT, scalar2=None,
                                             op0=ALU.mult), r=["anw"], w=["anw"])
            tk.dve(lambda e: e.tensor_tensor(out=lamp[:, 0, :], in0=lam4[:, 0, :], in1=lam4[:, 1, :], op=ALU.mult),
                   r=["lam4_0", "lam4_1"], w=["lamp0"])
            tk.dve(lambda e: e.tensor_tensor(out=lamp[:, 1, :], in0=lam4[:, 2, :], in1=lam4[:, 3, :], op=ALU.mult),
                   r=["lam4_2", "lam4_3"], w=["lamp1"])
            tk.dve(lambda e: e.tensor_reduce(out=lams[:, 0:2], in_=lamp[:], axis=AX.X, op=ALU.add),
                   r=["lamp0", "lamp1"], w=["lams"])
            tk.act(lambda e: e.activation(out=lams[:, 2:4], in_=lams[:, 0:2], func=AF.Exp), r=["lams"], w=["lams2"])
            tk.dve(lambda e: e.scalar_tensor_tensor(out=neglam[:], in0=lams[:, 3:4], scalar=-LAM_INIT,
                                                    in1=lams[:, 2:3], op0=ALU.add, op1=ALU.subtract),
                   r=["lams2"], w=["neglam"])
            for j in range(4):
                for c in range(8):
                    tk.dve(lambda e, j=j, c=c: e.tensor_scalar(out=convdiag[:, j * 8 + c, :], in0=ident_f[:],
                                                               scalar1=cw[:, j, c:c + 1], scalar2=None, op0=ALU.mult),
                           r=["cw"], w=["cd%d_%d" % (j, c)])
            engs = ["dve", "act", "pool"]
            for kc in range(8):
                st = stg[kc % 2]
                sk = "stg%d" % (kc % 2)
                tk.dma("sp", lambda e, kc=kc, st=st: e.dma_start(out=st[:], in_=w_in[0, kc * 128:(kc + 1) * 128, :]),
                       w=[sk])
                for part in range(3):
                    lo, hi = part * 1200, min(3592, (part + 1) * 1200)
                    en = engs[part]
                    if en == "act":
                        tk.act(lambda e, kc=kc, st=st, lo=lo, hi=hi: e.activation(
                            out=win[:, kc, lo:hi], in_=st[:, lo:hi], func=AF.Copy, scale=n1[:, kc:kc + 1]),
                            r=[sk, "n1"], w=["win%d_%d" % (kc, part)])
                    else:
                        tk.op(en, lambda e, kc=kc, st=st, lo=lo, hi=hi: e.tensor_scalar(
                            out=win[:, kc, lo:hi], in0=st[:, lo:hi], scalar1=n1[:, kc:kc + 1], scalar2=None,
                            op0=ALU.mult), r=[sk, "n1"], w=["win%d_%d" % (kc, part)])
            for kc in range(8):
                st = stg[kc % 2]
                sk = "stg%d" % (kc % 2)
                tk.dma("sp", lambda e, kc=kc, st=st: e.dma_start(out=st[:, 0:1024],
                                                                 in_=w_out[0, kc * 128:(kc + 1) * 128, :]), w=[sk])
                sc = mnw if kc < 4 else anw
                tk.dve(lambda e, kc=kc, st=st, sc=sc: e.tensor_scalar(out=wout[:, kc, :], in0=st[:, 0:1024],
                                                                      scalar1=sc[:, 0:1], scalar2=None, op0=ALU.mult),
                       r=[sk, "mnw", "anw"], w=["wout%d" % kc])
            tk.barrier()

        xb = [sbt(sa, "xb%d" % i, [128, 1024], F32) for i in range(3)]
        junk = sbt(sa, "junk", [128, 1024], BF16)
        hb = sbt(sa, "hb", [128, 1024], BF16)
        hT = sbt(sa, "hT", [128, 8, 128], BF16)
        qkpre = sbt(sa, "qkpre", [128, 8, 131], BF16)
        qkT = sbt(sa, "qkT", [128, 8, 128], BF16)
        vm = sbt(sa, "vm", [128, 4, 129], BF16)
        og = sbt(sa, "og", [128, 512], F32)
        gt = sbt(sa, "gt", [4, 4, 128], F32)
        R = sbt(sa, "R", [4, 3, 128], F32)
        Rt = sbt(sa, "Rt", [4, 3, 128], F32)
        Rhi = sbt(sa, "Rhi", [4, 3, 128], BF16)
        Rlo = sbt(sa, "Rlo", [4, 3, 128], BF16)
        gcar = sbt(sa, "gcar", [4, 8], F32)
        gc = sbt(sa, "gc", [128, 12], F32)
        sTp = sbt(sa, "sTp", [128, 4, 128], BF16)
        kw = sbt(sa, "kw", [128, 4, 128], BF16)
        Cst = sbt(sa, "Cst", [128, 4, 129], F32)
        Cbf = sbt(sa, "Cbf", [128, 4, 129], BF16)
        mtmp = sbt(sa, "mtmp", [128, 16], F32)
        rtmp = sbt(sa, "rtmp", [128, 16], F32)
        dtmp = sbt(sa, "dtmp", [128, 8], F32)
        atmp = sbt(sa, "atmp", [128, 512], F32)
        hm = sbt(sa, "hm", [128, 1024], F32)
        ss8 = sbt(sa, "ss8", [128, 8], F32)
        rs8 = sbt(sa, "rs8", [128, 8], F32)
        cat = sbt(sa, "cat", [128, 1024], BF16)
        catT = sbt(sa, "catT", [128, 8, 128], BF16)
        qkn = sbt(sa, "qkn", [128, 1024], BF16)
        ssq = sbt(sa, "ssq", [128, 16], F32)
        rsq = sbt(sa, "rsq", [128, 16], F32)
        sq = sbt(sa, "sq", [128, 1024], F32)
        qT = sbt(sa, "qT", [128, 4, 128], BF16)
        kc_ = sbt(sa, "kcache", [128, 4, S], BF16)
        vc_ = sbt(sa, "vcache", [128, TPS, 4, 129], BF16)
        pT = [sbt(sa, "pT%d" % i, [128, 512], BF16) for i in range(3)]
        oraw = sbt(sa, "oraw", [128, 4, 2, 129], F32)
        arec = sbt(sa, "arec", [128, 4, 2], F32)
        x1 = sbt(sa, "x1", [128, 1024], F32)
        sx = sbt(sa, "sx", [128, 8], F32)
        h2f = sq
        h2hi = sbt(sa, "h2hi", [128, 1024], BF16)
        h2lo = sbt(sa, "h2lo", [128, 1024], BF16)
        h2T = [sbt(sa, "h2T%d" % i, [128, 8, 128], BF16) for i in range(2)]
        lg = sbt(sa, "lg", [128, 36], F32)
        rt = sbt(sa, "rt", [128, 64], F32)
        rt3 = sbt(sa, "rt3", [128, 4, 8], F32)
        m8 = sbt(sa, "m8", [128, 8], F32)

        tk.dve(lambda e: e.memset(vm[:], 1.0), w=["vm"])
        tk.dve(lambda e: e.memset(vc_[:], 1.0), w=["vcall"])
        tk.barrier()

        def bfv(i):
            return ps[i][:].bitcast(BF16)

        def transposes8(src, dst_fn, srckey, post, n=8):
            b = bank()
            v = bfv(b)
            for k in range(n):
                tk.pe(lambda e, k=k: e.transpose(out=v[:, k * 128:(k + 1) * 128], in_=src[:, k * 128:(k + 1) * 128],
                                                 identity=ident_b[:]),
                      r=(srckey if isinstance(srckey, list) else [srckey]), w=["ps%d" % b])
            post(v, "ps%d" % b)

        def rstd(ss_ap, tmp_ap, out_ap, invd, rk, wk):
            tk.dve(lambda e: e.tensor_scalar(out=tmp_ap, in0=ss_ap, scalar1=invd, scalar2=EPS, op0=ALU.mult,
                                             op1=ALU.add), r=[rk], w=[wk + "_t"])
            tk.act(lambda e: e.activation(out=tmp_ap, in_=tmp_ap, func=AF.Sqrt), r=[wk + "_t"], w=[wk + "_t"])
            tk.dve(lambda e: e.reciprocal(out=out_ap, in_=tmp_ap), r=[wk + "_t"], w=[wk])

        pre = []
        for ex in range(NE):
            pre.append((wbf[ex * 128:(ex + 1) * 128, 0:4096].rearrange("p (kc n) -> p kc n", kc=8),
                        w_gate[0, ex].rearrange("(kc p) n -> p kc n", p=128)))
            pre.append((wbf[ex * 128:(ex + 1) * 128, 4096:8192].rearrange("p (kc n) -> p kc n", kc=8),
                        w_up[0, ex].rearrange("(kc p) n -> p kc n", p=128)))
            pre.append((wbf[ex * 128:(ex + 1) * 128, 8192:12288].rearrange("p (kc n) -> p kc n", kc=4),
                        w_down[0, ex].rearrange("(kc p) n -> p kc n", p=128)))
        pre_i = [0]

        def issue_pre(n):
            for _ in range(n):
                if pre_i[0] < len(pre):
                    o_, i_ = pre[pre_i[0]]
                    tk.dma("pool", lambda e, o_=o_, i_=i_: e.dma_start(out=o_, in_=i_), w=["pre%d" % pre_i[0]])
                    pre_i[0] += 1

        def router_t(ti):
            for i_, (src, sk) in enumerate(((h2hi, "h2hi"), (h2lo, "h2lo"))):
                transposes8(src, None, sk, lambda v, pk, i_=i_: tk.dve(
                    lambda e: e.tensor_copy(out=h2T[i_][:].rearrange("p a b -> p (a b)"), in_=v[:, 0:1024]),
                    r=[pk], w=["h2T%d" % i_]))

        def router_m1(ti):
            bl = bank()
            combos = [(0, wr_hi), (0, wr_lo), (1, wr_hi)]
            n_mm = 0
            for i_, wr in combos:
                for k in range(8):
                    tk.pe(lambda e, i_=i_, wr=wr, k=k, n_mm=n_mm: e.matmul(ps[bl][:, 0:36], lhsT=h2T[i_][:, k, :],
                                                                           rhs=wr[:, k, :], start=(n_mm == 0),
                                                                           stop=(n_mm == 23)),
                          r=["h2T0", "h2T1"], w=["ps%d" % bl])
                    n_mm += 1
            tk.dve(lambda e: e.tensor_tensor(out=lg[:], in0=ps[bl][:, 0:36], in1=rbias[:], op=ALU.add),
                   r=["ps%d" % bl], w=["lg"])
            tk.dve(lambda e: e.tensor_reduce(out=rt[:, 0:1], in_=lg[:, 0:4], axis=AX.X, op=ALU.max), r=["lg"], w=["rt0"])
            tk.dve(lambda e: e.tensor_scalar(out=rt[:, 4:8], in0=lg[:, 0:4], scalar1=rt[:, 0:1], scalar2=None,
                                             op0=ALU.is_equal), r=["lg", "rt0"], w=["gone"])
            tk.dve(lambda e: e.tensor_scalar(out=rt[:, 1:2], in0=rt[:, 0:1], scalar1=-1.0, scalar2=None, op0=ALU.mult),
                   r=["rt0"], w=["rt1"])
            tk.dve(lambda e: e.tensor_tensor(out=rt3[:], in0=lg[:, 4:36].rearrange("p (g x) -> p g x", g=4),
                                             in1=rt[:, 4:8].unsqueeze(2).to_broadcast([128, 4, 8]), op=ALU.mult),
                   r=["lg", "gone"], w=["rt3"])
            tk.dve(lambda e: e.tensor_reduce(out=rt[:, 16:24], in_=rt3[:].rearrange("p g x -> p x g"), axis=AX.X,
                                             op=ALU.add), r=["rt3"], w=["ein"])
            tk.dve(lambda e: e.tensor_tensor(out=rt[:, 12:16], in0=rt[:, 4:8], in1=iota32[:, 0:4], op=ALU.mult),
                   r=["gone"], w=["gi4"])
            tk.dve(lambda e: e.tensor_reduce(out=rt[:, 24:25], in_=rt[:, 12:16], axis=AX.X, op=ALU.add), r=["gi4"], w=["gidx"])
            tk.dve(lambda e: e.max(out=m8[:], in_=rt[:, 16:24]), r=["ein"], w=["m8"])
            for k_, (eid, rw) in enumerate(((eid1, rw1), (eid2, rw2))):
                tk.dve(lambda e, k_=k_: e.tensor_scalar(out=rt[:, 32:40], in0=rt[:, 16:24], scalar1=m8[:, k_:k_ + 1],
                                                        scalar2=None, op0=ALU.is_equal), r=["ein", "m8"], w=["msk"])
                tk.dve(lambda e: e.tensor_tensor(out=rt[:, 32:40], in0=rt[:, 32:40], in1=iota32[:, 0:8], op=ALU.mult),
                       r=["msk"], w=["msk"])
                tk.dve(lambda e: e.tensor_reduce(out=rt[:, 25:26], in_=rt[:, 32:40], axis=AX.X, op=ALU.add),
                       r=["msk"], w=["eidx"])
                tk.dve(lambda e, eid=eid: e.scalar_tensor_tensor(out=eid[:, ti:ti + 1], in0=rt[:, 24:25], scalar=8.0,
                                                                 in1=rt[:, 25:26], op0=ALU.mult, op1=ALU.add),
                       r=["gidx", "eidx"], w=["eid"])
            tk.dve(lambda e: e.tensor_tensor(out=rt[:, 26:27], in0=m8[:, 1:2], in1=m8[:, 0:1], op=ALU.subtract),
                   r=["m8"], w=["dd"])

        def router_m2(ti):
            tk.act(lambda e: e.activation(out=rt[:, 8:12], in_=lg[:, 0:4], func=AF.Exp, bias=rt[:, 1:2],
                                          accum_out=rt[:, 2:3]), r=["lg", "rt1"], w=["rt2"])
            tk.dve(lambda e: e.reciprocal(out=rt[:, 3:4], in_=rt[:, 2:3]), r=["rt2"], w=["gw"])
            tk.act(lambda e: e.activation(out=rt[:, 27:28], in_=rt[:, 26:27], func=AF.Exp), r=["dd"], w=["ex"])
            tk.dve(lambda e: e.tensor_scalar(out=rt[:, 28:29], in0=rt[:, 27:28], scalar1=1.0, scalar2=None, op0=ALU.add),
                   r=["ex"], w=["ex1"])
            tk.dve(lambda e: e.reciprocal(out=rt[:, 29:30], in_=rt[:, 28:29]), r=["ex1"], w=["p1"])
            tk.dve(lambda e: e.tensor_tensor(out=rw1[:, ti:ti + 1], in0=rt[:, 29:30], in1=rt[:, 3:4], op=ALU.mult),
                   r=["p1", "gw"], w=["rw1"])
            tk.dve(lambda e: e.tensor_tensor(out=rw2[:, ti:ti + 1], in0=rw1[:, ti:ti + 1], in1=rt[:, 27:28], op=ALU.mult),
                   r=["rw1", "ex"], w=["rw2"])


        def P1a(ti):
            qt = ti % TPS
            xt = xb[ti % 3]
            xk = "xb%d" % (ti % 3)
            tk.act(lambda e: e.activation(out=junk[:], in_=xt[:], func=AF.Square, accum_out=sx[:, 0:1]),
                   r=[xk], w=["sx0"])
            rstd(sx[:, 0:1], sx[:, 1:2], sx[:, 2:3], 1.0 / 1024, "sx0", "sx2")
            tk.act(lambda e: e.activation(out=hb[:], in_=xt[:], func=AF.Copy, scale=sx[:, 2:3]),
                   r=[xk, "sx2"], w=["hb"])

        def P1b(ti):
            qt = ti % TPS
            xt = xb[ti % 3]
            xk = "xb%d" % (ti % 3)
            transposes8(hb, None, "hb", lambda v, pk: tk.dve(
                lambda e: e.tensor_copy(out=hT[:].rearrange("p a b -> p (a b)"), in_=v[:, 0:1024]), r=[pk], w=["hT"]))

        def P2(ti):
            qt = ti % TPS
            xt = xb[ti % 3]
            xk = "xb%d" % (ti % 3)
            bg = bank()
            for gi, col in enumerate((2048, 2052)):
                for k in range(8):
                    tk.pe(lambda e, gi=gi, col=col, k=k: e.matmul(ps[bg][0:4, gi * 128:(gi + 1) * 128],
                                                                  lhsT=win[:, k, col:col + 4], rhs=hT[:, k, :],
                                                                  start=(k == 0), stop=(k == 7)), r=["hT"], w=["ps%d" % bg])
            if qt == 0:
                tk.dve(lambda e: e.memset(gcar[:, 0:2], 0.0), w=["gcar"])
                tk.dve(lambda e: e.memset(Cst[:], 0.0), w=["Cst"])
            tk.act(lambda e: e.activation(out=gt[:, 0, :], in_=ps[bg][0:4, 128:256], func=AF.Exp, bias=gbias[:, 2:3],
                                          scale=-1.0), r=["ps%d" % bg], w=["gt0"])
            tk.dve(lambda e: e.tensor_scalar(out=gt[:, 0, :], in0=gt[:, 0, :], scalar1=1.0, scalar2=None, op0=ALU.add),
                   r=["gt0"], w=["gt0"])
            tk.act(lambda e: e.activation(out=gt[:, 1, :], in_=gt[:, 0, :], func=AF.Ln), r=["gt0"], w=["gt1"])
            tk.dve(lambda e: e.tensor_tensor_scan(out=gt[:, 2, :], data0=gt[:, 1, :], data1=zeros_f[0:4, :],
                                                  initial=gcar[:, 0:1], op0=ALU.add, op1=ALU.add),
                   r=["gt1", "gcar"], w=["gt2"])
            tk.dve(lambda e: e.scalar_tensor_tensor(out=gt[:, 3, :], in0=ps[bg][0:4, 0:128], scalar=gbias[:, 0:1],
                                                    in1=gt[:, 2, :], op0=ALU.add, op1=ALU.add),
                   r=["ps%d" % bg, "gt2"], w=["gt3"])
            tk.dve(lambda e: e.tensor_reduce(out=gcar[:, 2:3], in_=gt[:, 3, :], axis=AX.X, op=ALU.max),
                   r=["gt3"], w=["gcar2"])
            tk.dve(lambda e: e.tensor_tensor(out=gcar[:, 3:4], in0=gcar[:, 1:2], in1=gcar[:, 2:3], op=ALU.max),
                   r=["gcar", "gcar2"], w=["gcar3"])
            tk.dve(lambda e: e.tensor_scalar(out=gcar[:, 4:5], in0=gcar[:, 3:4], scalar1=-1.0,
                                             scalar2=-0.5 * math.log(128.0), op0=ALU.mult, op1=ALU.add),
                   r=["gcar3"], w=["gcar4"])
            tk.dve(lambda e: e.tensor_scalar(out=gcar[:, 5:6], in0=gcar[:, 3:4], scalar1=-1.0, scalar2=None,
                                             op0=ALU.mult), r=["gcar3"], w=["gcar5"])
            tk.dve(lambda e: e.tensor_tensor(out=gcar[:, 6:7], in0=gcar[:, 1:2], in1=gcar[:, 3:4], op=ALU.subtract),
                   r=["gcar", "gcar3"], w=["gcar6"])
            tk.act(lambda e: e.activation(out=R[:, 0, :], in_=gt[:, 3, :], func=AF.Exp, bias=gcar[:, 4:5]),
                   r=["gt3", "gcar4"], w=["R0"])
            tk.act(lambda e: e.activation(out=R[:, 1, :], in_=gt[:, 2, :], func=AF.Exp, bias=gcar[:, 5:6]),
                   r=["gt2", "gcar5"], w=["R1"])
            tk.act(lambda e: e.activation(out=R[:, 2, :], in_=zeros_f[0:4, :], func=AF.Exp, bias=gcar[:, 6:7]),
                   r=["gcar6"], w=["R2"])
            tk.dve(lambda e: e.tensor_copy(out=gcar[:, 0:1], in_=gt[:, 2, 127:128]), r=["gt2"], w=["gcar"])
            tk.dve(lambda e: e.tensor_copy(out=gcar[:, 1:2], in_=gcar[:, 3:4]), r=["gcar3", "gcar6"], w=["gcar"])
            tk.dve(lambda e: e.tensor_copy(out=Rhi[:], in_=R[:]), r=["R0", "R1", "R2"], w=["Rhi"])
            tk.dve(lambda e: e.tensor_copy(out=Rt[:], in_=Rhi[:]), r=["Rhi"], w=["Rt"])
            tk.dve(lambda e: e.tensor_tensor(out=Rlo[:], in0=R[:], in1=Rt[:], op=ALU.subtract),
                   r=["R0", "R1", "R2", "Rt"], w=["Rlo"])
            if qt == 0:
                tk.dve(lambda e: e.memset(qkpre[:, :, 0:3], 0.0), w=["qkpre"])
            else:
                tk.dve(lambda e: e.tensor_copy(out=qkpre[:, :, 0:3], in_=qkpre[:, :, 128:131]), r=["qkpre"], w=["qkpre"])
            for half in range(2):
                b = bank()
                for cc in range(4):
                    c = half * 4 + cc
                    for k in range(8):
                        tk.pe(lambda e, c=c, cc=cc, k=k, b=b: e.matmul(
                            ps[b][:, cc * 128:(cc + 1) * 128], lhsT=win[:, k, c * 128:(c + 1) * 128], rhs=hT[:, k, :],
                            start=(k == 0), stop=(k == 7)), r=["hT"], w=["ps%d" % b])
                tk.dve(lambda e, half=half, b=b: e.tensor_copy(
                    out=qkpre[:, half * 4:(half + 1) * 4, 3:131],
                    in_=ps[b][:].rearrange("p (c t) -> p c t", c=4)), r=["ps%d" % b], w=["qkpre"])

        def P3a(ti):
            qt = ti % TPS
            xt = xb[ti % 3]
            xk = "xb%d" % (ti % 3)
            bmv, bmo = bank(), bank()
            for b, col in ((bmv, 1024), (bmo, 1536)):
                for k in range(8):
                    tk.pe(lambda e, b=b, col=col, k=k: e.matmul(ps[b][:], lhsT=hT[:, k, :], rhs=win[:, k, col:col + 512],
                                                                 start=(k == 0), stop=(k == 7)), r=["hT"], w=["ps%d" % b])
            tk.dve(lambda e: e.tensor_copy(out=vm[:, :, 0:128], in_=ps[bmv][:].rearrange("p (h d) -> p h d", h=4)),
                   r=["ps%d" % bmv], w=["vm"])
            tk.act(lambda e: e.activation(out=og[:], in_=ps[bmo][:], func=AF.Sigmoid), r=["ps%d" % bmo], w=["og"])
            bq, bk = bank(), bank()
            for b, col in ((bq, 2056), (bk, 2568)):
                for k in range(8):
                    tk.pe(lambda e, b=b, col=col, k=k: e.matmul(ps[b][:], lhsT=hT[:, k, :], rhs=win[:, k, col:col + 512],
                                                                 start=(k == 0), stop=(k == 7)), r=["hT"], w=["ps%d" % b])
            for i_, b in enumerate((bq, bk)):
                tk.act(lambda e, i_=i_, b=b: e.activation(out=sq[:, i_ * 512:(i_ + 1) * 512], in_=ps[b][:], func=AF.Square),
                       r=["ps%d" % b], w=["sq"])
                tk.act(lambda e, i_=i_, b=b: e.activation(out=qkn[:, i_ * 512:(i_ + 1) * 512], in_=ps[b][:], func=AF.Copy),
                       r=["ps%d" % b], w=["qkn"])
            tk.dve(lambda e: e.tensor_reduce(out=ssq[:], in_=sq[:].rearrange("p (g d) -> p g d", d=64), axis=AX.X,
                                             op=ALU.add), r=["sq"], w=["ssq"])
            rstd(ssq[:], rtmp[:], rsq[:], 1.0 / 64, "ssq", "rsq")
            for i_, b in enumerate((bq, bk)):
                tk.dve(lambda e, i_=i_, b=b: e.tensor_tensor(
                    out=qkn[:, i_ * 512:(i_ + 1) * 512].rearrange("p (g d) -> p g d", d=64),
                    in0=qkn[:, i_ * 512:(i_ + 1) * 512].rearrange("p (g d) -> p g d", d=64),
                    in1=rsq[:, i_ * 8:(i_ + 1) * 8].unsqueeze(2).to_broadcast([128, 8, 64]), op=ALU.mult),
                    r=["qkn", "rsq"], w=["qkn"])
            bv = bank()
            for k in range(8):
                tk.pe(lambda e, k=k: e.matmul(ps[bv][:], lhsT=hT[:, k, :], rhs=win[:, k, 3080:3592],
                                              start=(k == 0), stop=(k == 7)), r=["hT"], w=["ps%d" % bv])
            tk.act(lambda e: e.activation(out=vc_[:, qt, :, 0:128], in_=ps[bv][:].rearrange("p (h d) -> p h d", h=4),
                                          func=AF.Copy), r=["ps%d" % bv], w=["vc%d" % qt])

        def P3b1(ti):
            qt = ti % TPS
            xt = xb[ti % 3]
            xk = "xb%d" % (ti % 3)
            for half in range(2):
                b = bank()
                for cc in range(4):
                    c = half * 4 + cc
                    for j in range(4):
                        tk.pe(lambda e, c=c, cc=cc, j=j, b=b: e.matmul(
                            ps[b][:, cc * 128:(cc + 1) * 128], lhsT=convdiag[:, j * 8 + c, :], rhs=qkpre[:, c, j:j + 128],
                            start=(j == 0), stop=(j == 3)), r=["qkpre"], w=["ps%d" % b])
                for cc in range(4):
                    c = half * 4 + cc
                    tk.act(lambda e, c=c, cc=cc, b=b: e.activation(out=qkT[:, c, :], in_=ps[b][:, cc * 128:(cc + 1) * 128],
                                                                   func=AF.Silu, bias=convb[:, c:c + 1]),
                           r=["ps%d" % b], w=["qkT%d" % c])

        def P3b2(ti):
            qt = ti % TPS
            xt = xb[ti % 3]
            xk = "xb%d" % (ti % 3)
            bc = bank()
            for bi in range(3):
                tk.pe(lambda e, bi=bi: e.matmul(ps[bc][:, bi * 4:(bi + 1) * 4], lhsT=Rhi[:, bi, :], rhs=ident_b[0:4, 0:4],
                                                start=True, stop=False), r=["Rhi"], w=["ps%d" % bc])
                tk.pe(lambda e, bi=bi: e.matmul(ps[bc][:, bi * 4:(bi + 1) * 4], lhsT=Rlo[:, bi, :], rhs=ident_b[0:4, 0:4],
                                                start=False, stop=True), r=["Rlo"], w=["ps%d" % bc])
            tk.dve(lambda e: e.tensor_copy(out=gc[:], in_=ps[bc][:, 0:12]), r=["ps%d" % bc], w=["gc"])
            bs = bank()
            for h in range(4):
                tk.pe(lambda e, h=h: e.matmul(ps[bs][:, h * 128:(h + 1) * 128], lhsT=qkT[:, 4 + h, :], rhs=qkT[:, h, :],
                                              start=True, stop=True), r=["qkT%d" % h, "qkT%d" % (4 + h)], w=["ps%d" % bs])
            for h in range(4):
                tk.dve(lambda e, h=h: e.scalar_tensor_tensor(out=sTp[:, h, :], in0=ps[bs][:, h * 128:(h + 1) * 128],
                                                             scalar=gc[:, h:h + 1], in1=tri[:], op0=ALU.mult,
                                                             op1=ALU.mult), r=["ps%d" % bs, "gc"], w=["sTp%d" % h])
            bt = bank()
            vt = bfv(bt)
            for h in range(4):
                tk.pe(lambda e, h=h: e.transpose(out=vt[:, h * 128:(h + 1) * 128], in_=qkT[:, 4 + h, :], identity=ident_b[:]),
                      r=["qkT%d" % (4 + h)], w=["ps%d" % bt])
            for h in range(4):
                tk.act(lambda e, h=h: e.activation(out=kw[:, h, :], in_=vt[:, h * 128:(h + 1) * 128], func=AF.Copy,
                                                   scale=gc[:, h:h + 1]), r=["ps%d" % bt, "gc"], w=["kw%d" % h])
            for h in range(4):
                tk.dve(lambda e, h=h: e.tensor_scalar(out=Cst[:, h, :], in0=Cst[:, h, :], scalar1=gc[:, 8 + h:9 + h],
                                                      scalar2=None, op0=ALU.mult), r=["Cst", "gc"], w=["Cst"])
            tk.dve(lambda e: e.tensor_copy(out=Cbf[:], in_=Cst[:]), r=["Cst"], w=["Cbf"])
            bn = [bank(), bank()]
            for h in range(4):
                o_ = ps[bn[h // 2]][:, (h % 2) * 129:(h % 2) * 129 + 129]
                tk.pe(lambda e, h=h, o_=o_: e.matmul(o_, lhsT=sTp[:, h, :], rhs=vm[:, h, :], start=True, stop=False),
                      r=["sTp%d" % h, "vm"], w=["ps%d" % bn[h // 2]])
                tk.pe(lambda e, h=h, o_=o_: e.matmul(o_, lhsT=qkT[:, h, :], rhs=Cbf[:, h, :], start=False, stop=True),
                      r=["qkT%d" % h, "Cbf"], w=["ps%d" % bn[h // 2]])
            bu = [bank(), bank()]
            for h in range(4):
                o_ = ps[bu[h // 2]][:, (h % 2) * 129:(h % 2) * 129 + 129]
                tk.pe(lambda e, h=h, o_=o_: e.matmul(o_, lhsT=kw[:, h, :], rhs=vm[:, h, :], start=True, stop=True),
                      r=["kw%d" % h, "vm"], w=["ps%d" % bu[h // 2]])
            for h2_ in range(2):
                tk.dve(lambda e, h2_=h2_: e.tensor_tensor(
                    out=Cst[:, 2 * h2_:2 * h2_ + 2, :], in0=Cst[:, 2 * h2_:2 * h2_ + 2, :],
                    in1=ps[bu[h2_]][:, 0:258].rearrange("p (h d) -> p h d", h=2), op=ALU.add),
                    r=["Cst", "ps%d" % bu[h2_], "Cbf"], w=["Cst"])
            for h in range(4):
                nd = ps[bn[h // 2]]
                off = (h % 2) * 129
                pk = "ps%d" % bn[h // 2]
                tk.dve(lambda e, h=h, nd=nd, off=off: e.tensor_copy(out=dtmp[:, h:h + 1], in_=nd[:, off + 128:off + 129]),
                       r=[pk], w=["dt%d" % h])
                tk.dve(lambda e, h=h: e.scalar_tensor_tensor(
                    out=mtmp[:, h:h + 1], in0=dtmp[:, h:h + 1], scalar=-1.0, in1=dtmp[:, h:h + 1],
                    op0=ALU.mult, op1=ALU.max), r=["dt%d" % h], w=["mt%d" % h])
                tk.dve(lambda e, h=h: e.tensor_tensor(out=mtmp[:, h:h + 1], in0=mtmp[:, h:h + 1], in1=gc[:, 4 + h:5 + h],
                                                      op=ALU.max), r=["mt%d" % h, "gc"], w=["mt%d" % h])
                tk.dve(lambda e, h=h: e.reciprocal(out=mtmp[:, 4 + h:5 + h], in_=mtmp[:, h:h + 1]),
                       r=["mt%d" % h], w=["mr%d" % h])
                tk.dve(lambda e, h=h, nd=nd, off=off: e.scalar_tensor_tensor(
                    out=hm[:, h * 128:(h + 1) * 128], in0=nd[:, off:off + 128], scalar=mtmp[:, 4 + h:5 + h],
                    in1=og[:, h * 128:(h + 1) * 128], op0=ALU.mult, op1=ALU.mult),
                    r=[pk, "mr%d" % h, "og"], w=["hm%d" % h])
                tk.act(lambda e, h=h: e.activation(out=junk[:, h * 128:(h + 1) * 128], in_=hm[:, h * 128:(h + 1) * 128],
                                                   func=AF.Square, accum_out=ss8[:, h:h + 1]),
                       r=["hm%d" % h], w=["ss8_%d" % h])
            def post_qk(v, pk):
                tk.dve(lambda e: e.tensor_scalar(out=qT[:].rearrange("p a b -> p (a b)"), in0=v[:, 0:512],
                                                 scalar1=nqk[:, 0:1], scalar2=None, op0=ALU.mult), r=[pk], w=["qT"])
                tk.act(lambda e: e.activation(out=kc_[:, :, qt * 128:(qt + 1) * 128],
                                              in_=v[:, 512:1024].rearrange("p (h t) -> p h t", h=4), func=AF.Copy,
                                              scale=nqk[:, 1:2]), r=[pk], w=["kc%d" % qt])
            transposes8(qkn, None, "qkn", post_qk)

        def P4(ti):
            qt = ti % TPS
            xt = xb[ti % 3]
            xk = "xb%d" % (ti % 3)
            nk = qt + 1
            items = []
            for h in range(4):
                for c in range(2):
                    for g0 in range(0, nk, 4):
                        items.append((h, c, list(range(g0, min(nk, g0 + 4)))))
            pti = [0]

            def qk_exp(it):
                h, c, kts = it
                b = bank()
                p_ = pti[0] % 3
                pti[0] += 1
                for j, kt in enumerate(kts):
                    tk.pe(lambda e, j=j, kt=kt: e.matmul(ps[b][:, j * 128:(j + 1) * 128],
                                                         lhsT=kc_[c * 64:(c + 1) * 64, h, kt * 128:(kt + 1) * 128],
                                                         rhs=qT[c * 64:(c + 1) * 64, h, :], start=True, stop=True),
                          r=["kc%d" % kt, "qT"], w=["ps%d" % b])
                n = len(kts)
                tk.act(lambda e: e.activation(out=pT[p_][:, 0:n * 128], in_=ps[b][:, 0:n * 128], func=AF.Exp, scale=0.125),
                       r=["ps%d" % b], w=["pT%d" % p_])
                if kts[-1] == qt:
                    j = n - 1
                    tk.pool(lambda e: e.memset(pT[p_][64:128, j * 128:j * 128 + 64], 0.0), w=["pT%d" % p_])
                return p_

            def pv(it, p_):
                h, c, kts = it
                pa = 6 + (h % 2)
                for j, kt in enumerate(kts):
                    tk.pe(lambda e, j=j, kt=kt: e.matmul(ps[pa][:, c * 129:(c + 1) * 129], lhsT=pT[p_][:, j * 128:(j + 1) * 128],
                                                         rhs=vc_[:, kt, h, :], start=(kt == 0), stop=(kt == qt)),
                          r=["pT%d" % p_, "vc%d" % kt], w=["ps%d" % pa])
                if c == 1 and kts[-1] == qt:
                    tk.act(lambda e: e.activation(out=oraw[:, h, :, :].rearrange("p a b -> p (a b)"), in_=ps[pa][:, 0:258],
                                                  func=AF.Copy), r=["ps%d" % pa], w=["oraw%d" % h])
                    tk.dve(lambda e: e.reciprocal(out=arec[:, h, :], in_=oraw[:, h, :, 128]), r=["oraw%d" % h], w=["arec%d" % h])
                    tk.dve(lambda e: e.tensor_scalar(out=arec[:, h, 1:2], in0=arec[:, h, 1:2], scalar1=neglam[:, 0:1],
                                                     scalar2=None, op0=ALU.mult), r=["arec%d" % h], w=["arec%d" % h])
                    tk.dve(lambda e: e.tensor_scalar(out=atmp[:, h * 128:(h + 1) * 128], in0=oraw[:, h, 0, 0:128],
                                                     scalar1=arec[:, h, 0:1], scalar2=None, op0=ALU.mult),
                           r=["oraw%d" % h, "arec%d" % h], w=["sqa%d" % h])
                    tk.dve(lambda e: e.scalar_tensor_tensor(out=hm[:, 512 + h * 128:512 + (h + 1) * 128],
                                                            in0=oraw[:, h, 1, 0:128], scalar=arec[:, h, 1:2],
                                                            in1=atmp[:, h * 128:(h + 1) * 128], op0=ALU.mult, op1=ALU.add),
                           r=["oraw%d" % h, "arec%d" % h, "sqa%d" % h], w=["hm%d" % (4 + h)])
                    tk.act(lambda e: e.activation(out=junk[:, 512 + h * 128:512 + (h + 1) * 128],
                                                  in_=hm[:, 512 + h * 128:512 + (h + 1) * 128], func=AF.Square,
                                                  accum_out=ss8[:, 4 + h:5 + h]), r=["hm%d" % (4 + h)],
                           w=["ss8_%d" % (4 + h)])

            pend_pv = []
            for it in items:
                p_ = qk_exp(it)
                pend_pv.append((it, p_))
                if len(pend_pv) > 2:
                    pv(*pend_pv.pop(0))
            for pp in pend_pv:
                pv(*pp)

        def P5a(ti):
            qt = ti % TPS
            xt = xb[ti % 3]
            xk = "xb%d" % (ti % 3)
            tk.dve(lambda e: e.tensor_scalar(out=mtmp[:, 8:16], in0=ss8[:], scalar1=1.0 / 128, scalar2=EPS, op0=ALU.mult,
                                             op1=ALU.add), r=["ss8_%d" % i for i in range(8)], w=["mt8"])
            tk.act(lambda e: e.activation(out=mtmp[:, 8:16], in_=mtmp[:, 8:16], func=AF.Sqrt), r=["mt8"], w=["mt8"])
            tk.dve(lambda e: e.reciprocal(out=rs8[:], in_=mtmp[:, 8:16]), r=["mt8"], w=["rs8"])
            tk.dve(lambda e: e.tensor_tensor(out=cat[:].rearrange("p (g d) -> p g d", d=128),
                                             in0=hm[:].rearrange("p (g d) -> p g d", d=128),
                                             in1=rs8[:].unsqueeze(2).to_broadcast([128, 8, 128]), op=ALU.mult),
                   r=["hm%d" % i for i in range(8)] + ["rs8"], w=["cat"])

        def P5b(ti):
            qt = ti % TPS
            xt = xb[ti % 3]
            xk = "xb%d" % (ti % 3)
            transposes8(cat, None, "cat", lambda v, pk: tk.act(
                lambda e: e.activation(out=catT[:].rearrange("p a b -> p (a b)"), in_=v[:, 0:1024], func=AF.Copy),
                r=[pk], w=["catT"]))
            by = [bank(), bank()]
            for half in range(2):
                for k in range(8):
                    tk.pe(lambda e, half=half, k=k: e.matmul(ps[by[half]][:], lhsT=catT[:, k, :],
                                                             rhs=wout[:, k, half * 512:(half + 1) * 512],
                                                             start=(k == 0), stop=(k == 7)), r=["catT"], w=["ps%d" % by[half]])
            for half in range(2):
                tk.dve(lambda e, half=half: e.tensor_tensor(out=x1[:, half * 512:(half + 1) * 512], in0=ps[by[half]][:],
                                                            in1=xt[:, half * 512:(half + 1) * 512], op=ALU.add),
                       r=["ps%d" % by[half], xk], w=["x1_%d" % half])
            tk.dma("sp", lambda e: e.dma_start(out=x1buf[ti * 128:(ti + 1) * 128, :], in_=x1[:]),
                   r=["x1_0", "x1_1"], w=["x1buf%d" % ti])
            tk.act(lambda e: e.activation(out=junk[:], in_=x1[:], func=AF.Square, accum_out=sx[:, 4:5]),
                   r=["x1_0", "x1_1"], w=["sx4"])
            rstd(sx[:, 4:5], sx[:, 5:6], sx[:, 3:4], 1.0 / 1024, "sx4", "sx3")
            tk.dve(lambda e: e.scalar_tensor_tensor(out=h2f[:], in0=x1[:], scalar=sx[:, 3:4], in1=n2w[:], op0=ALU.mult,
                                                    op1=ALU.mult), r=["x1_0", "x1_1", "sx3"], w=["sq"])
            tk.act(lambda e: e.activation(out=h2hi[:], in_=h2f[:], func=AF.Copy), r=["sq"], w=["h2hi"])
            tk.dma("sp", lambda e: e.dma_start(out=h2buf[ti * 128:(ti + 1) * 128, :], in_=h2hi[:]), r=["h2hi"],
                   w=["h2buf%d" % ti])
            tk.dve(lambda e: e.tensor_tensor(out=h2lo[:], in0=h2f[:], in1=h2hi[:], op=ALU.subtract),
                   r=["sq", "h2hi"], w=["h2lo"])

        for t0 in range(min(2, NT)):
            tk.dma("sp", lambda e, t0=t0: e.dma_start(out=xb[t0 % 3][:], in_=x[t0 * 128:(t0 + 1) * 128, :]), w=["xb%d" % (t0 % 3)])
        P1a(0)
        P1b(0)
        P2(0)
        P3a(0)
        for ti in range(NT):
            if ti + 2 < NT:
                tk.dma("sp", lambda e, ti=ti: e.dma_start(out=xb[(ti + 2) % 3][:], in_=x[(ti + 2) * 128:(ti + 3) * 128, :]),
                       w=["xb%d" % ((ti + 2) % 3)])
            issue_pre(2)
            if ti + 1 < NT:
                P1a(ti + 1)
            P3b1(ti)
            if ti > 0:
                router_t(ti - 1)
            if ti + 1 < NT:
                P1b(ti + 1)
            P3b2(ti)
            if ti > 0:
                router_m1(ti - 1)
            P4(ti)
            if ti > 0:
                router_m2(ti - 1)
            P5a(ti)
            if ti + 1 < NT:
                P2(ti + 1)
                P3a(ti + 1)
            P5b(ti)
        router_t(NT - 1)
        router_m1(NT - 1)
        router_m2(NT - 1)
        issue_pre(len(pre))
        tk.barrier()

    NCOL = NT * 32
    CH = [(c0, min(NCOL, c0 + 512)) for c0 in range(0, NCOL, 512)]
    assert 2 * len(CH) <= 8
    sb_ = ExitStack()
    dest1 = sbt(sb_, "dest1", [128, NT], I32)
    dest2 = sbt(sb_, "dest2", [128, NT], I32)
    widx = sbt(sb_, "widx", [128, NB], I32)
    with ExitStack() as s2:
        oh1 = sbt(s2, "oh1", [128, NT, 32], F32)
        oh2 = sbt(s2, "oh2", [128, NT, 32], F32)
        comb = sbt(s2, "comb", [128, NT, 32], BF16)
        base = sbt(s2, "base", [128, NT, 32], F32)
        cumA = sbt(s2, "cumA", [128, NT, 32], F32)
        cumB = sbt(s2, "cumB", [128, NT, 32], F32)
        tot = sbt(s2, "tot", [128, NT, 32], F32)
        cnt = sbt(s2, "cnt", [128, 32], F32)
        cmpb = sbt(s2, "cmpb", [128, 32, MAXB], F32)
        thr = sbt(s2, "thr", [128, MAXB], F32)
        nblk = sbt(s2, "nblk", [128, 32], F32)
        pend = sbt(s2, "pend", [128, 32], F32)
        pstart = sbt(s2, "pstart", [128, 32], F32)
        jB = sbt(s2, "jB", [128, NB], F32)
        cmpe = sbt(s2, "cmpe", [128, NB, 32], F32)
        bef = sbt(s2, "bef", [128, NB], F32)
        dstf = sbt(s2, "dstf", [128, 2, NT], F32)
        tokf = sbt(s2, "tokf", [128, NT], F32)
        toki = sbt(s2, "toki", [128, NT, 16], I32)
        initi = sbt(s2, "initi", [128, NG * 16], I32)
        iot = iota32[:].unsqueeze(1).to_broadcast([128, NT, 32])
        tk.dve(lambda e: e.tensor_tensor(out=oh1[:], in0=eid1[:].unsqueeze(2).to_broadcast([128, NT, 32]), in1=iot,
                                         op=ALU.is_equal), w=["oh1"])
        tk.dve(lambda e: e.tensor_tensor(out=oh2[:], in0=eid2[:].unsqueeze(2).to_broadcast([128, NT, 32]), in1=iot,
                                         op=ALU.is_equal), w=["oh2"])
        tk.dve(lambda e: e.tensor_tensor(out=comb[:], in0=oh1[:], in1=oh2[:], op=ALU.add), r=["oh1", "oh2"], w=["comb"])
        combf = comb[:].rearrange("p a b -> p (a b)")
        for ci, (c0, c1) in enumerate(CH):
            tk.pe(lambda e, ci=ci, c0=c0, c1=c1: e.matmul(ps[ci][:, 0:c1 - c0], lhsT=ustrict[:], rhs=combf[:, c0:c1],
                                                          start=True, stop=True), r=["comb"], w=["ps%d" % ci])
            tk.pe(lambda e, ci=ci, c0=c0, c1=c1: e.matmul(ps[4 + ci][:, 0:c1 - c0], lhsT=ones_b[:], rhs=combf[:, c0:c1],
                                                          start=True, stop=True), r=["comb"], w=["ps%d" % (4 + ci)])
        basef = base[:].rearrange("p a b -> p (a b)")
        totf = tot[:].rearrange("p a b -> p (a b)")
        for ci, (c0, c1) in enumerate(CH):
            tk.dve(lambda e, ci=ci, c0=c0, c1=c1: e.tensor_copy(out=basef[:, c0:c1], in_=ps[ci][:, 0:c1 - c0]),
                   r=["ps%d" % ci], w=["base"])
            tk.act(lambda e, ci=ci, c0=c0, c1=c1: e.activation(out=totf[:, c0:c1], in_=ps[4 + ci][:, 0:c1 - c0],
                                                               func=AF.Copy), r=["ps%d" % (4 + ci)], w=["tot"])
        src, dst = tot, cumA
        d = 1
        while d < NT:
            tk.dve(lambda e, src=src, dst=dst, d=d: e.tensor_copy(out=dst[:, 0:d, :], in_=src[:, 0:d, :]),
                   r=["tot", "cumA", "cumB"], w=["cumA" if dst is cumA else "cumB"])
            tk.dve(lambda e, src=src, dst=dst, d=d: e.tensor_tensor(out=dst[:, d:NT, :], in0=src[:, d:NT, :],
                                                                    in1=src[:, 0:NT - d, :], op=ALU.add),
                   r=["tot", "cumA", "cumB"], w=["cumA" if dst is cumA else "cumB"])
            src = dst
            dst = cumB if dst is cumA else cumA
            d *= 2
        inc = src
        inck = "tot" if inc is tot else ("cumA" if inc is cumA else "cumB")
        tk.dve(lambda e: e.tensor_copy(out=cnt[:], in_=inc[:, NT - 1, :]), r=[inck], w=["cnt"])
        tk.dve(lambda e: e.tensor_tensor(out=base[:], in0=base[:], in1=inc[:], op=ALU.add), r=["base", inck], w=["base"])
        tk.dve(lambda e: e.tensor_tensor(out=base[:], in0=base[:], in1=tot[:], op=ALU.subtract), r=["base", "tot"], w=["base"])
        tk.pool(lambda e: e.iota(thr[:], pattern=[[B, MAXB]], base=0, channel_multiplier=0,
                                 allow_small_or_imprecise_dtypes=True), w=["thr"])
        tk.pool(lambda e: e.iota(jB[:], pattern=[[B, NB]], base=0, channel_multiplier=0,
                                 allow_small_or_imprecise_dtypes=True), w=["jB"])
        tk.pool(lambda e: e.iota(tokf[:], pattern=[[128, NT]], base=0, channel_multiplier=1,
                                 allow_small_or_imprecise_dtypes=True), w=["tokf"])
        tk.dve(lambda e: e.tensor_tensor(out=cmpb[:], in0=cnt[:].unsqueeze(2).to_broadcast([128, 32, MAXB]),
                                         in1=thr[:].unsqueeze(1).to_broadcast([128, 32, MAXB]), op=ALU.is_gt),
               r=["cnt", "thr"], w=["cmpb"])
        tk.dve(lambda e: e.tensor_reduce(out=nblk[:], in_=cmpb[:], axis=AX.X, op=ALU.add), r=["cmpb"], w=["nblk"])
        tk.dve(lambda e: e.tensor_scalar(out=nblk[:], in0=nblk[:], scalar1=float(B), scalar2=None, op0=ALU.mult),
               r=["nblk"], w=["nblk"])
        tk.dve(lambda e: e.tensor_tensor_scan(out=pend[:], data0=nblk[:], data1=zeros_f[:, 0:32], initial=0.0,
                                              op0=ALU.add, op1=ALU.add), r=["nblk"], w=["pend"])
        tk.dve(lambda e: e.tensor_tensor(out=pstart[:], in0=pend[:], in1=nblk[:], op=ALU.subtract),
               r=["pend", "nblk"], w=["pstart"])
        tk.dve(lambda e: e.tensor_tensor(out=base[:], in0=base[:], in1=pstart[:].unsqueeze(1).to_broadcast([128, NT, 32]),
                                         op=ALU.add), r=["base", "pstart"], w=["base"])
        for k_, (oh, dd) in enumerate(((oh1, dest1), (oh2, dest2))):
            tk.dve(lambda e, oh=oh: e.tensor_tensor(out=oh[:], in0=oh[:], in1=base[:], op=ALU.mult),
                   r=["base", "oh1", "oh2", "comb"], w=["oh%d" % (k_ + 1)])
            tk.dve(lambda e, oh=oh, k_=k_: e.tensor_reduce(out=dstf[:, k_, :], in_=oh[:], axis=AX.X, op=ALU.add),
                   r=["oh%d" % (k_ + 1)], w=["dstf%d" % k_])
            tk.dve(lambda e, dd=dd, k_=k_: e.tensor_copy(out=dd[:], in_=dstf[:, k_, :]), r=["dstf%d" % k_], w=["dest%d" % k_])
        tk.dve(lambda e: e.tensor_tensor(out=cmpe[:], in0=pend[:].unsqueeze(1).to_broadcast([128, NB, 32]),
                                         in1=jB[:].unsqueeze(2).to_broadcast([128, NB, 32]), op=ALU.is_le),
               r=["pend", "jB"], w=["cmpe"])
        tk.dve(lambda e: e.tensor_reduce(out=bef[:], in_=cmpe[:], axis=AX.X, op=ALU.add), r=["cmpe"], w=["bef"])
        tk.dve(lambda e: e.tensor_scalar(out=bef[:], in0=bef[:], scalar1=float(NE - 1), scalar2=128.0, op0=ALU.min,
                                         op1=ALU.mult), r=["bef"], w=["bef"])
        if dbg:
            tk.dma("sp", lambda e: e.dma_start(out=dbg_be, in_=bef[:]), r=["bef"], w=["dbg_be"])
            tk.dma("sp", lambda e: e.dma_start(out=dbg_d, in_=dstf[:].rearrange("p a b -> p (a b)")),
                   r=["dstf0", "dstf1"], w=["dbg_d"])
            tk.dma("sp", lambda e: e.dma_start(out=dbg_r[:, 0:NT], in_=eid1[:]), w=["dbg_r0"])
            tk.dma("sp", lambda e: e.dma_start(out=dbg_r[:, NT:2 * NT], in_=eid2[:]), w=["dbg_r1"])
            tk.dma("sp", lambda e: e.dma_start(out=dbg_r[:, 2 * NT:3 * NT], in_=rw1[:]), w=["dbg_r2"])
            tk.dma("sp", lambda e: e.dma_start(out=dbg_r[:, 3 * NT:4 * NT], in_=rw2[:]), w=["dbg_r3"])
        tk.dve(lambda e: e.tensor_scalar(out=bef[:], in0=bef[:], scalar1=pidx[:, 0:1], scalar2=None, op0=ALU.add),
               r=["bef", "dbg_be"], w=["bef"])
        tk.dve(lambda e: e.tensor_copy(out=widx[:], in_=bef[:]), r=["bef"], w=["widx"])
        tk.dve(lambda e: e.memset(toki[:], 0), w=["toki"])
        tk.dve(lambda e: e.tensor_copy(out=toki[:, :, 0], in_=tokf[:]), r=["tokf", "toki"], w=["toki"])
        tk.dve(lambda e: e.memset(initi[:], T), w=["initi"])
        tk.dma("sp", lambda e: e.dma_start(out=slot_tok.rearrange("(p g) c -> p (g c)", p=128), in_=initi[:]),
               r=["initi"], w=["slot_tok"])
        for ti in range(NT):
            for dd, dk in ((dest1, "dest0"), (dest2, "dest1")):
                tk.dma("pool", lambda e, ti=ti, dd=dd: e.indirect_dma_start(
                    out=slot_tok, out_offset=bass.IndirectOffsetOnAxis(ap=dd[:, ti:ti + 1], axis=0),
                    in_=toki[:, ti, :], in_offset=None, bounds_check=bnds["slot"], oob_is_err=False),
                    r=[dk, "toki", "slot_tok"], w=["st_%d_%s" % (ti, dk)])
        tk.barrier()

    with ExitStack() as s3:
        wblk = [sbt(s3, "wblk%d" % i, [128, 12288], BF16) for i in range(2)]
        gidx_all = sbt(s3, "gidx_all", [128, NG], I32)
        for g in range(NG):
            tk.dma("sp", lambda e, g=g: e.dma_start(out=gidx_all[:, g:g + 1], in_=slot_tok[g * 128:(g + 1) * 128, 0:1],
                                                    allow_slow_non_contiguous=True), w=["gidx%d" % g])
        xg = [sbt(s3, "xg%d" % i, [128, 1024], BF16) for i in range(4)]
        xT = [sbt(s3, "xT%d" % i, [128, 8, B], BF16) for i in range(2)]
        sg = [sbt(s3, "sg%d" % i, [128, B], F32) for i in range(2)]
        hid = [sbt(s3, "hid%d" % i, [128, 4, B], BF16) for i in range(2)]
        yst = [sbt(s3, "yst%d" % i, [128, 1024], F32) for i in range(2)]
        gcount = 0
        ycount = 0
        for j in range(NB):
            wb = wblk[j % 2]
            wk = "wblk%d" % (j % 2)
            tk.dma("pool", lambda e, j=j, wb=wb: e.indirect_dma_start(
                out=wb[:], out_offset=None, in_=wbf, in_offset=bass.IndirectOffsetOnAxis(ap=widx[:, j:j + 1], axis=0),
                bounds_check=bnds["w"], oob_is_err=False), w=[wk])
            xTj = xT[j % 2]
            xTk = "xT%d" % (j % 2)
            for gi in range(GB):
                g = j * GB + gi
                s_ = gcount % 4
                gcount += 1
                tk.dma("pool", lambda e, s_=s_, g=g: e.indirect_dma_start(
                    out=xg[s_][:], out_offset=None, in_=h2buf,
                    in_offset=bass.IndirectOffsetOnAxis(ap=gidx_all[:, g:g + 1], axis=0), bounds_check=bnds["h"],
                    oob_is_err=False), r=["gidx%d" % g], w=["xg%d" % s_])
                b = bank()
                v = ps[b][:].bitcast(BF16)
                for k in range(8):
                    tk.pe(lambda e, k=k, s_=s_: e.transpose(out=v[:, k * 128:(k + 1) * 128],
                                                            in_=xg[s_][:, k * 128:(k + 1) * 128], identity=ident_b[:]),
                          r=["xg%d" % s_], w=["ps%d" % b])
                tk.dve(lambda e, gi=gi, v=v: e.tensor_copy(out=xTj[:, :, gi * 128:(gi + 1) * 128],
                                                           in_=v[:, 0:1024].rearrange("p (k t) -> p k t", k=8)),
                       r=["ps%d" % b], w=[xTk + "_%d" % gi])
            hj = hid[j % 2]
            hk = "hid%d" % (j % 2)
            for hc in range(4):
                b = bank()
                for m in range(2):
                    for k in range(8):
                        tk.pe(lambda e, m=m, k=k, hc=hc, b=b: e.matmul(
                            ps[b][:, m * B:(m + 1) * B], lhsT=wb[:, m * 4096 + k * 512 + hc * 128:m * 4096 + k * 512 + hc * 128 + 128],
                            rhs=xTj[:, k, :], start=(k == 0), stop=(k == 7)),
                            r=[wk] + [xTk + "_%d" % gi for gi in range(GB)], w=["ps%d" % b])
                sgi = sg[hc % 2]
                tk.act(lambda e, b=b, sgi=sgi: e.activation(out=sgi[:], in_=ps[b][:, 0:B], func=AF.Silu),
                       r=["ps%d" % b], w=["sg%d" % (hc % 2)])
                tk.dve(lambda e, b=b, sgi=sgi, hc=hc: e.tensor_tensor(out=hj[:, hc, :], in0=sgi[:], in1=ps[b][:, B:2 * B],
                                                                      op=ALU.mult),
                       r=["ps%d" % b, "sg%d" % (hc % 2)], w=[hk + "_%d" % hc])
            for gi in range(GB):
                ys = yst[ycount % 2]
                yk = "yst%d" % (ycount % 2)
                ycount += 1
                for half in range(2):
                    b = bank()
                    for hc in range(4):
                        tk.pe(lambda e, hc=hc, half=half, b=b, gi=gi: e.matmul(
                            ps[b][:], lhsT=hj[:, hc, gi * 128:(gi + 1) * 128],
                            rhs=wb[:, 8192 + hc * 1024 + half * 512:8192 + hc * 1024 + half * 512 + 512],
                            start=(hc == 0), stop=(hc == 3)), r=[wk] + [hk + "_%d" % i for i in range(4)], w=["ps%d" % b])
                    if half == 0:
                        tk.act(lambda e, b=b, ys=ys: e.activation(out=ys[:, 0:512], in_=ps[b][:], func=AF.Copy),
                               r=["ps%d" % b], w=[yk + "a"])
                    else:
                        tk.dve(lambda e, b=b, ys=ys: e.tensor_copy(out=ys[:, 512:1024], in_=ps[b][:]),
                               r=["ps%d" % b], w=[yk + "b"])
                row0 = (j * GB + gi) * 128
                tk.dma("sp", lambda e, ys=ys, row0=row0: e.dma_start(out=ybuf[row0:row0 + 128, :], in_=ys[:]),
                       r=[yk + "a", yk + "b"], w=["ybuf%d" % row0])
        tk.barrier()

    with ExitStack() as s4:
        x1t = [sbt(s4, "x1t%d" % i, [128, 1024], F32) for i in range(2)]
        y1t = [sbt(s4, "y1t%d" % i, [128, 1024], F32) for i in range(2)]
        y2t = [sbt(s4, "y2t%d" % i, [128, 1024], F32) for i in range(2)]
        tk.dma("sp", lambda e: e.dma_start(out=x1t[0][:], in_=x1buf[0:128, :]), w=["x1t0"])
        for ti in range(NT):
            s_ = ti % 2
            if ti + 1 < NT:
                tk.dma("sp", lambda e, ti=ti: e.dma_start(out=x1t[(ti + 1) % 2][:], in_=x1buf[(ti + 1) * 128:(ti + 2) * 128, :]),
                       w=["x1t%d" % ((ti + 1) % 2)])
            for yt, dd, nm in ((y1t, dest1, "y1t"), (y2t, dest2, "y2t")):
                tk.dma("pool", lambda e, ti=ti, s_=s_, yt=yt, dd=dd: e.indirect_dma_start(
                    out=yt[s_][:], out_offset=None, in_=ybuf,
                    in_offset=bass.IndirectOffsetOnAxis(ap=dd[:, ti:ti + 1], axis=0), bounds_check=bnds["slot"],
                    oob_is_err=False), w=["%s%d" % (nm, s_)])
            tk.dve(lambda e, ti=ti, s_=s_: e.scalar_tensor_tensor(out=x1t[s_][:], in0=y1t[s_][:], scalar=rw1[:, ti:ti + 1],
                                                                  in1=x1t[s_][:], op0=ALU.mult, op1=ALU.add),
                   r=["y1t%d" % s_, "x1t%d" % s_], w=["x1t%d" % s_])
            tk.dve(lambda e, ti=ti, s_=s_: e.scalar_tensor_tensor(out=x1t[s_][:], in0=y2t[s_][:], scalar=rw2[:, ti:ti + 1],
                                                                  in1=x1t[s_][:], op0=ALU.mult, op1=ALU.add),
                   r=["y2t%d" % s_, "x1t%d" % s_], w=["x1t%d" % s_])
            tk.dma("sp", lambda e, ti=ti, s_=s_: e.dma_start(out=out[ti * 128:(ti + 1) * 128, :], in_=x1t[s_][:]),
                   r=["x1t%d" % s_], w=["out%d" % ti])
        tk.barrier()
    sb_.close()
    cs.close()
    return nc, tk


PARAMS = ["norm1_w", "w_in", "b_igate", "b_fgate", "conv_w", "conv_b", "m_norm_w", "q_norm_w", "k_norm_w",
          "lambda_q1", "lambda_k1", "lambda_q2", "lambda_k2", "a_norm_w", "w_out", "norm2_w", "w_group", "b_group",
          "w_expert", "b_expert", "w_gate", "w_up", "w_down"]


def run(inputs, n_cores, NSEQ, S, dbg=False, trace=False):
    nc, tk = build(NSEQ=NSEQ, S=S, dbg=dbg)
    xs = np.ascontiguousarray(inputs["x"], dtype=np.float32).reshape(n_cores, NSEQ * S, 1024)
    shared = {k: np.ascontiguousarray(inputs[k], dtype=np.float32) for k in PARAMS}
    in_maps = []
    for c in range(n_cores):
        m = dict(shared)
        m["x"] = xs[c]
        in_maps.append(m)
    res = run_bass_kernel_spmd(nc, in_maps, core_ids=list(range(n_cores)), **({"trace": True} if trace else {}))
    return res


def kernel(**inputs):
    res = run(inputs, 8, 4, 2048)
    outs = [np.asarray(r["out"]).reshape(4, 2048, 1024) for r in res.results]
    return np.concatenate(outs, axis=0).astype(np.float32)
```
